# Optimizing a Trainium2 kernel written in Bass

```python
import jax, jax.numpy as jnp
from jax import lax
import numpy as np

D_MODEL = 1024
BATCH = 8
SEQ = 2048
DEPTH = 2

GRID_W = 64
CTX_LEN = 256
EPS = 1e-6

ATTN_HEADS = 8
ATTN_KV_HEADS = 2
HEAD_DIM = 64
ATTN_REP = ATTN_HEADS // ATTN_KV_HEADS
ATTN_W = ATTN_HEADS * HEAD_DIM
KV_W = ATTN_KV_HEADS * HEAD_DIM
ROPE_THETA = 10000.0
Q_BLOCK = 128

CONV_CH = D_MODEL // 2
CONV_WIDTH = 31

IN_EVEN = ATTN_W + 2 * KV_W + 2 * CONV_CH
MIX_EVEN = ATTN_W + CONV_CH

D_INNER = 2 * D_MODEL
SSM_HEADDIM = 64
SSM_HEADS = D_INNER // SSM_HEADDIM
SSM_GROUPS = 4
HEADS_PER_GROUP = SSM_HEADS // SSM_GROUPS
D_STATE = 128
SSM_CONV = 7
CHUNK = 128
GN = SSM_GROUPS * D_STATE
CONV_DIM = D_INNER + 2 * GN
IN_ODD = D_INNER + CONV_DIM + 2 * SSM_HEADS

D_FF = 2816
N_EXPERTS = 8
TOP_K = 2
D_FF_EXPERT = 3584

N_EVEN = (DEPTH + 1) // 2
N_ODD = DEPTH // 2

kernel_name = 'hybrid_dit_attn_conv_ssd_moe'


def rms_norm(x, g):
    xf = x.astype(jnp.float32)
    y = xf * lax.rsqrt(jnp.mean(xf * xf, axis=-1, keepdims=True) + EPS)
    return (y * g.astype(jnp.float32)).astype(x.dtype)


def layer_norm(x, g, b):
    xf = x.astype(jnp.float32)
    mu = jnp.mean(xf, axis=-1, keepdims=True)
    var = jnp.mean(jnp.square(xf - mu), axis=-1, keepdims=True)
    y = (xf - mu) * lax.rsqrt(var + EPS)
    return (y * g.astype(jnp.float32) + b.astype(jnp.float32)).astype(x.dtype)


def adaln(cond, w, b):
    m = jax.nn.silu(cond) @ w + b
    return jnp.split(m[..., None, :], 6, axis=-1)


def modulate(h, shift, scale):
    return h * (1 + scale) + shift


def depthwise_conv(x, w, b):
    width = w.shape[0]
    y = lax.conv_general_dilated(x, w[:, None, :].astype(x.dtype), window_strides=(1,),
                                 padding=[(width // 2, width // 2)],
                                 dimension_numbers=('NWC', 'WIO', 'NWC'),
                                 feature_group_count=x.shape[-1])
    return y + b


def axial_rope(n_tokens):
    rows = n_tokens // GRID_W
    t_row = jnp.repeat(jnp.arange(rows, dtype=jnp.float32), GRID_W)
    t_col = jnp.tile(jnp.arange(GRID_W, dtype=jnp.float32), rows)
    axis_dim = HEAD_DIM // 2
    inv_freq = ROPE_THETA ** (-jnp.arange(0, axis_dim, 2, dtype=jnp.float32) / axis_dim)
    ang = jnp.concatenate([t_row[:, None] * inv_freq, t_col[:, None] * inv_freq], axis=-1)
    return jnp.cos(ang)[:, None, :], jnp.sin(ang)[:, None, :]


def apply_rope(x, cos, sin):
    xf = x.astype(jnp.float32).reshape(x.shape[:-1] + (HEAD_DIM // 2, 2))
    x1, x2 = xf[..., 0], xf[..., 1]
    out = jnp.stack([x1 * cos - x2 * sin, x1 * sin + x2 * cos], axis=-1)
    return out.reshape(x.shape).astype(x.dtype)


def attend(q, k, v):
    s = jnp.einsum('bqkrd,bskd->bkrqs', q, k).astype(jnp.float32) * (HEAD_DIM ** -0.5)
    p = jax.nn.softmax(s, axis=-1).astype(v.dtype)
    return jnp.einsum('bkrqs,bskd->bqkrd', p, v)


def latent_attention(q, k, v, k_ctx, v_ctx):
    b, t = q.shape[:2]
    nb = t // Q_BLOCK
    k_all = jnp.concatenate([k_ctx, k], axis=1)
    v_all = jnp.concatenate([v_ctx, v], axis=1)
    qb = q.reshape(b, nb, Q_BLOCK, ATTN_KV_HEADS, ATTN_REP, HEAD_DIM).swapaxes(0, 1)
    o = lax.map(lambda qblk: attend(qblk, k_all, v_all), qb)
    return o.swapaxes(0, 1).reshape(b, t, ATTN_W)


def context_attention(q, k, v):
    b, t = q.shape[:2]
    return attend(q.reshape(b, t, ATTN_KV_HEADS, ATTN_REP, HEAD_DIM), k, v).reshape(b, t, ATTN_W)


def even_proj(h, w_in, q_gain, k_gain):
    b, t = h.shape[:2]
    q, k, v, u = jnp.split(h @ w_in, [ATTN_W, ATTN_W + KV_W, ATTN_W + 2 * KV_W], axis=-1)
    q = rms_norm(q.reshape(b, t, ATTN_HEADS, HEAD_DIM), q_gain)
    k = rms_norm(k.reshape(b, t, ATTN_KV_HEADS, HEAD_DIM), k_gain)
    return q, k, v.reshape(b, t, ATTN_KV_HEADS, HEAD_DIM), u


def conformer_conv(u, dw_w, dw_b, ln_g, ln_b):
    a, g = jnp.split(u, 2, axis=-1)
    h = depthwise_conv(a * jax.nn.sigmoid(g), dw_w, dw_b)
    return jax.nn.silu(layer_norm(h, ln_g, ln_b))


def even_mixer(h, hc, w_in, q_gain, k_gain, dw_w, dw_b, ln_g, ln_b, w_o, need_ctx):
    q, k, v, u = even_proj(h, w_in, q_gain, k_gain)
    qc, kc, vc, uc = even_proj(hc, w_in, q_gain, k_gain)
    cos, sin = axial_rope(h.shape[1])
    q = apply_rope(q, cos, sin)
    k = apply_rope(k, cos, sin)
    a_lat = latent_attention(q, k, v, kc, vc)
    b_lat = conformer_conv(u, dw_w, dw_b, ln_g, ln_b)
    y = jnp.concatenate([a_lat, b_lat], axis=-1) @ w_o
    if not need_ctx:
        return y, None
    a_ctx = context_attention(qc, kc, vc)
    b_ctx = conformer_conv(uc, dw_w, dw_b, ln_g, ln_b)
    yc = jnp.concatenate([a_ctx, b_ctx], axis=-1) @ w_o
    return y, yc


def ssm_proj(h, w_in, conv_w, conv_b):
    b, t = h.shape[:2]
    z, xbc, dt = jnp.split(h @ w_in, [D_INNER, D_INNER + CONV_DIM], axis=-1)
    xbc = jax.nn.silu(depthwise_conv(xbc, conv_w, conv_b))
    xs, bm, cm = jnp.split(xbc, [D_INNER, D_INNER + GN], axis=-1)
    dt_f, dt_b = jnp.split(dt, 2, axis=-1)
    hs = (b, t, SSM_GROUPS, HEADS_PER_GROUP)
    return (z, xs.reshape(hs + (SSM_HEADDIM,)), bm.reshape(b, t, SSM_GROUPS, D_STATE),
            cm.reshape(b, t, SSM_GROUPS, D_STATE), dt_f.reshape(hs), dt_b.reshape(hs))


def ssd_scan(x, dt, a_coef, bm, cm, init_state, return_y):
    b, t = x.shape[:2]
    nc = t // CHUNK
    xc = (x.astype(jnp.float32) * dt[..., None]).reshape(b, nc, CHUNK, SSM_GROUPS, HEADS_PER_GROUP, SSM_HEADDIM)
    bc = bm.astype(jnp.float32).reshape(b, nc, CHUNK, SSM_GROUPS, D_STATE)
    cc = cm.astype(jnp.float32).reshape(b, nc, CHUNK, SSM_GROUPS, D_STATE)
    a = (dt * a_coef).reshape(b, nc, CHUNK, SSM_GROUPS, HEADS_PER_GROUP)
    a_cs = jnp.cumsum(jnp.moveaxis(a, 2, -1), axis=-1)
    decay_to_end = jnp.exp(a_cs[..., -1:] - a_cs)
    chunk_states = jnp.einsum('bclgn,bcgel,bclgep->bcgepn', bc, decay_to_end, xc)
    chunk_decay = jnp.exp(a_cs[..., -1])

    def carry(state, inp):
        s_c, d_c = inp
        return state * d_c[..., None, None] + s_c, state

    final, start = lax.scan(carry, init_state.astype(jnp.float32),
                            (jnp.moveaxis(chunk_states, 1, 0), jnp.moveaxis(chunk_decay, 1, 0)))
    if not return_y:
        return None, final
    start = jnp.moveaxis(start, 0, 1)
    lower = jnp.tril(jnp.ones((CHUNK, CHUNK), dtype=bool))
    seg = jnp.exp(jnp.where(lower, a_cs[..., :, None] - a_cs[..., None, :], -jnp.inf))
    cb = jnp.einsum('bclgn,bcsgn->bcgls', cc, bc)
    y_diag = jnp.einsum('bcgls,bcgels,bcsgep->bclgep', cb, seg, xc)
    y_off = jnp.einsum('bclgn,bcgepn,bcgel->bclgep', cc, start, jnp.exp(a_cs))
    return (y_diag + y_off).reshape(x.shape), final


def bidir_ssd(xs, bm, cm, dt_f, dt_b, a_log_f, a_log_b, dt_bias_f, dt_bias_b, d_skip, init_f, init_b, return_y):
    shp = (SSM_GROUPS, HEADS_PER_GROUP)
    a_f = -jnp.exp(a_log_f.astype(jnp.float32)).reshape(shp)
    a_b = -jnp.exp(a_log_b.astype(jnp.float32)).reshape(shp)
    dtf = jax.nn.softplus(dt_f.astype(jnp.float32) + dt_bias_f.astype(jnp.float32).reshape(shp))
    dtb = jax.nn.softplus(dt_b.astype(jnp.float32) + dt_bias_b.astype(jnp.float32).reshape(shp))
    flip = lambda t: jnp.flip(t, axis=1)
    y_f, fin_f = ssd_scan(xs, dtf, a_f, bm, cm, init_f, return_y)
    y_b, fin_b = ssd_scan(flip(xs), flip(dtb), a_b, flip(bm), flip(cm), init_b, return_y)
    if not return_y:
        return None, fin_f, fin_b
    y = y_f + flip(y_b) + d_skip.astype(jnp.float32).reshape(shp + (1,)) * xs.astype(jnp.float32)
    return y.astype(xs.dtype), fin_f, fin_b


def odd_mixer(h, hc, w_in, conv_w, conv_b, a_log_f, a_log_b, dt_bias_f, dt_bias_b, d_skip, gnorm, w_out, need_ctx):
    ssm_p = (a_log_f, a_log_b, dt_bias_f, dt_bias_b, d_skip)
    z, xs, bm, cm, dtf, dtb = ssm_proj(h, w_in, conv_w, conv_b)
    zc, xsc, bmc, cmc, dtfc, dtbc = ssm_proj(hc, w_in, conv_w, conv_b)
    zero = jnp.zeros((h.shape[0], SSM_GROUPS, HEADS_PER_GROUP, SSM_HEADDIM, D_STATE), jnp.float32)
    yc, fin_f, fin_b = bidir_ssd(xsc, bmc, cmc, dtfc, dtbc, *ssm_p, zero, zero, need_ctx)
    y, _, _ = bidir_ssd(xs, bm, cm, dtf, dtb, *ssm_p, fin_f, fin_b, True)

    def gated_out(yy, zz):
        return rms_norm(yy.reshape(zz.shape) * jax.nn.silu(zz), gnorm) @ w_out

    if not need_ctx:
        return gated_out(y, z), None
    return gated_out(y, z), gated_out(yc, zc)


def swiglu(h, w1, w3, w2):
    return (jax.nn.silu(h @ w1) * (h @ w3)) @ w2


def moe_swiglu(h, w_router, w1, w3, w2):
    b, t, d = h.shape
    hf = h.reshape(b * t, d)
    logits = (hf @ w_router).astype(jnp.float32)
    top_v, top_i = lax.top_k(logits, TOP_K)
    top_w = jax.nn.softmax(top_v, axis=-1)
    gates = jnp.sum(jax.nn.one_hot(top_i, N_EXPERTS, dtype=jnp.float32) * top_w[..., None], axis=1)
    out = jnp.zeros_like(hf)
    for e in range(N_EXPERTS):
        out = out + gates[:, e:e + 1].astype(hf.dtype) * swiglu(hf, w1[e], w3[e], w2[e])
    return out.reshape(b, t, d)


def even_layer(x, xc, c, c_ctx, ada_w, ada_b, norm1, norm2, w_in, q_gain, k_gain, dw_w, dw_b, ln_g, ln_b,
               w_o, ff_w1, ff_w3, ff_w2, need_ctx):
    sh1, sc1, g1, sh2, sc2, g2 = adaln(c, ada_w, ada_b)
    csh1, csc1, cg1, csh2, csc2, cg2 = adaln(c_ctx, ada_w, ada_b)
    h = modulate(rms_norm(x, norm1), sh1, sc1)
    hc = modulate(rms_norm(xc, norm1), csh1, csc1)
    mix, mix_c = even_mixer(h, hc, w_in, q_gain, k_gain, dw_w, dw_b, ln_g, ln_b, w_o, need_ctx)
    x = x + g1 * mix
    x = x + g2 * swiglu(modulate(rms_norm(x, norm2), sh2, sc2), ff_w1, ff_w3, ff_w2)
    if not need_ctx:
        return x, None
    xc = xc + cg1 * mix_c
    xc = xc + cg2 * swiglu(modulate(rms_norm(xc, norm2), csh2, csc2), ff_w1, ff_w3, ff_w2)
    return x, xc


def odd_layer(x, xc, c, c_ctx, ada_w, ada_b, norm1, norm2, w_in, conv_w, conv_b, a_log_f, a_log_b,
              dt_bias_f, dt_bias_b, d_skip, gnorm, w_out, router, ex_w1, ex_w3, ex_w2, need_ctx):
    sh1, sc1, g1, sh2, sc2, g2 = adaln(c, ada_w, ada_b)
    csh1, csc1, cg1, csh2, csc2, cg2 = adaln(c_ctx, ada_w, ada_b)
    h = modulate(rms_norm(x, norm1), sh1, sc1)
    hc = modulate(rms_norm(xc, norm1), csh1, csc1)
    mix, mix_c = odd_mixer(h, hc, w_in, conv_w, conv_b, a_log_f, a_log_b, dt_bias_f, dt_bias_b, d_skip,
                           gnorm, w_out, need_ctx)
    x = x + g1 * mix
    x = x + g2 * moe_swiglu(modulate(rms_norm(x, norm2), sh2, sc2), router, ex_w1, ex_w3, ex_w2)
    if not need_ctx:
        return x, None
    xc = xc + cg1 * mix_c
    xc = xc + cg2 * moe_swiglu(modulate(rms_norm(xc, norm2), csh2, csc2), router, ex_w1, ex_w3, ex_w2)
    return x, xc


def setup_inputs(seed: int = 0) -> dict:
    key = jax.random.key(seed)
    ks = iter(jax.random.split(key, 64))

    def nrm(shape, scale):
        return jax.random.normal(next(ks), shape, jnp.float32) * scale

    def gain(shape):
        return 1.0 + nrm(shape, 0.02)

    def dt_bias(shape):
        dt = jnp.exp(jax.random.uniform(next(ks), shape, jnp.float32,
                                        minval=float(np.log(1e-3)), maxval=float(np.log(1e-1))))
        return dt + jnp.log(-jnp.expm1(-dt))

    def a_log(shape):
        return jnp.log(jax.random.uniform(next(ks), shape, jnp.float32, minval=1.0, maxval=16.0))

    ne, no = N_EVEN, N_ODD
    d = D_MODEL
    return {
        'x': nrm((BATCH, SEQ, d), 1.0),
        'c': nrm((BATCH, d), 1.0),
        'ctx': nrm((BATCH, CTX_LEN, d), 1.0),
        'c_ctx': nrm((d,), 1.0),
        'ev_ada_w': nrm((ne, d, 6 * d), 0.02),
        'ev_ada_b': nrm((ne, 6 * d), 0.02),
        'ev_norm1': gain((ne, d)),
        'ev_norm2': gain((ne, d)),
        'ev_w_in': nrm((ne, d, IN_EVEN), d ** -0.5),
        'ev_q_gain': gain((ne, HEAD_DIM)),
        'ev_k_gain': gain((ne, HEAD_DIM)),
        'ev_dw_w': nrm((ne, CONV_WIDTH, CONV_CH), CONV_WIDTH ** -0.5),
        'ev_dw_b': nrm((ne, CONV_CH), 0.02),
        'ev_ln_g': gain((ne, CONV_CH)),
        'ev_ln_b': nrm((ne, CONV_CH), 0.02),
        'ev_w_o': nrm((ne, MIX_EVEN, d), MIX_EVEN ** -0.5),
        'ev_ff_w1': nrm((ne, d, D_FF), d ** -0.5),
        'ev_ff_w3': nrm((ne, d, D_FF), d ** -0.5),
        'ev_ff_w2': nrm((ne, D_FF, d), D_FF ** -0.5),
        'od_ada_w': nrm((no, d, 6 * d), 0.02),
        'od_ada_b': nrm((no, 6 * d), 0.02),
        'od_norm1': gain((no, d)),
        'od_norm2': gain((no, d)),
        'od_w_in': nrm((no, d, IN_ODD), d ** -0.5),
        'od_conv_w': nrm((no, SSM_CONV, CONV_DIM), SSM_CONV ** -0.5),
        'od_conv_b': nrm((no, CONV_DIM), 0.02),
        'od_a_log_f': a_log((no, SSM_HEADS)),
        'od_a_log_b': a_log((no, SSM_HEADS)),
        'od_dt_bias_f': dt_bias((no, SSM_HEADS)),
        'od_dt_bias_b': dt_bias((no, SSM_HEADS)),
        'od_d_skip': gain((no, SSM_HEADS)),
        'od_gnorm': gain((no, D_INNER)),
        'od_w_out': nrm((no, D_INNER, d), D_INNER ** -0.5),
        'od_router': nrm((no, d, N_EXPERTS), d ** -0.5),
        'od_ex_w1': nrm((no, N_EXPERTS, d, D_FF_EXPERT), d ** -0.5),
        'od_ex_w3': nrm((no, N_EXPERTS, d, D_FF_EXPERT), d ** -0.5),
        'od_ex_w2': nrm((no, N_EXPERTS, D_FF_EXPERT, d), D_FF_EXPERT ** -0.5),
        'final_norm': gain((d,)),
    }


def reference(x, c, ctx, c_ctx,
              ev_ada_w, ev_ada_b, ev_norm1, ev_norm2, ev_w_in, ev_q_gain, ev_k_gain, ev_dw_w, ev_dw_b,
              ev_ln_g, ev_ln_b, ev_w_o, ev_ff_w1, ev_ff_w3, ev_ff_w2,
              od_ada_w, od_ada_b, od_norm1, od_norm2, od_w_in, od_conv_w, od_conv_b, od_a_log_f, od_a_log_b,
              od_dt_bias_f, od_dt_bias_b, od_d_skip, od_gnorm, od_w_out, od_router, od_ex_w1, od_ex_w3, od_ex_w2,
              final_norm):
    xc = ctx
    for i in range(DEPTH):
        need_ctx = i < DEPTH - 1
        j = i // 2
        if i % 2 == 0:
            x, xc = even_layer(x, xc, c, c_ctx, ev_ada_w[j], ev_ada_b[j], ev_norm1[j], ev_norm2[j], ev_w_in[j],
                               ev_q_gain[j], ev_k_gain[j], ev_dw_w[j], ev_dw_b[j], ev_ln_g[j], ev_ln_b[j],
                               ev_w_o[j], ev_ff_w1[j], ev_ff_w3[j], ev_ff_w2[j], need_ctx)
        else:
            x, xc = odd_layer(x, xc, c, c_ctx, od_ada_w[j], od_ada_b[j], od_norm1[j], od_norm2[j], od_w_in[j],
                              od_conv_w[j], od_conv_b[j], od_a_log_f[j], od_a_log_b[j], od_dt_bias_f[j],
                              od_dt_bias_b[j], od_d_skip[j], od_gnorm[j], od_w_out[j], od_router[j],
                              od_ex_w1[j], od_ex_w3[j], od_ex_w2[j], need_ctx)
    return rms_norm(x, final_norm)
```

```python
import numpy as np
from contextlib import ExitStack
import concourse.bass as bass
import concourse.mybir as mybir
from concourse.bass_utils import run_bass_kernel_spmd

F32 = mybir.dt.float32
BF16 = mybir.dt.bfloat16
ALU = mybir.AluOpType
AF = mybir.ActivationFunctionType
AX = mybir.AxisListType

D = 1024
T = 2048
TC = 256
TT = T + TC
NT = TT // 128
EPS = 1e-6
BLOCKS = [(0, 256)] + [(256 + 512 * i, 512) for i in range(4)]
LAT_BLOCKS = BLOCKS[1:]
D_FF = 2816
D_FFE = 3584
NEXP = 8


class Reg:
    __slots__ = ("w", "r")

    def __init__(self):
        self.w = []
        self.r = []


class Buf:
    def __init__(self, t, nreg=0):
        self.t = t
        self.reg = Reg()
        self.regs = [Reg() for _ in range(nreg)]

    def __getitem__(self, idx):
        return self.t[idx]


SEM_LIMIT = 12000


class KB:
    ENG = ("pe", "act", "dve", "pool", "sp")

    def __init__(self, nc, n_dma_sems=24):
        self.nc = nc
        self.es = ExitStack()
        self.e = {"pe": nc.tensor, "act": nc.scalar, "dve": nc.vector, "pool": nc.gpsimd, "sp": nc.sync}
        self.sems = {}
        self.owner = {}
        self.cur = {}
        self.nsem = 0
        for en in self.ENG:
            self._new_eng_sem(en)
        self.waited = {en: {} for en in self.ENG}
        self.dq = {}
        for q in ("sp", "pool"):
            lst = []
            for i in range(n_dma_sems):
                k = f"d_{q}{i}"
                self.sems[k] = self.es.enter_context(nc.semaphore(k))
                self.owner[k] = None
                lst.append([k, 0])
            self.dq[q] = [lst, 0]
        self.scopes = []
        self.ninst = 0

    def _new_eng_sem(self, en):
        k = f"s_{en}{self.nsem}"
        self.nsem += 1
        self.sems[k] = self.es.enter_context(self.nc.semaphore(k))
        self.owner[k] = en
        self.cur[en] = [k, 0]

    def scope(self):
        return _Scope(self)

    def _ctx(self):
        return self.scopes[-1] if self.scopes else self.es

    def sb(self, name, shape, dt, nreg=0):
        self.ninst += 0
        self.nalloc = getattr(self, "nalloc", 0) + 1
        name = f"{name}_{self.nalloc}"
        return Buf(self._ctx().enter_context(self.nc.sbuf_tensor(name, list(shape), dt)), nreg)

    def ps(self, name, shape, dt, nreg=0):
        return Buf(self._ctx().enter_context(self.nc.psum_tensor(name, list(shape), dt)), nreg)

    def dram(self, name, shape, dt, kind="Internal", nreg=0):
        t = self.nc.dram_tensor(name, list(shape), dt, kind=kind)
        return Buf(t.ap(), nreg)

    def _wait(self, en, evs):
        need = {}
        for (k, v) in evs:
            if self.owner[k] == en:
                ck, cv = self.cur[en]
                if en == "pe" or k != ck or v > cv:
                    continue
            if self.waited[en].get(k, 0) >= v:
                continue
            if need.get(k, 0) < v:
                need[k] = v
        for k, v in need.items():
            self.e[en].wait_ge(self.sems[k], v)
            self.waited[en][k] = v

    @staticmethod
    def _regs(lst):
        out = []
        for b in lst:
            if isinstance(b, Buf):
                out.append(b.reg)
            elif isinstance(b, (list, tuple)):
                out += KB._regs(b)
            else:
                out.append(b)
        return out

    def op(self, en, fn, reads=(), writes=(), signal=True):
        R = self._regs(reads)
        W = self._regs(writes)
        evs = []
        for r in R:
            evs += r.w
        for w in W:
            evs += w.w
            evs += w.r
        self._wait(en, evs)
        cur = self.cur[en]
        if signal and cur[1] >= SEM_LIMIT:
            self._new_eng_sem(en)
            cur = self.cur[en]
        ev = (cur[0], cur[1] + 1)
        inst = fn()
        self.ninst += 1
        if signal:
            inst.then_inc(self.sems[cur[0]], 1)
            cur[1] += 1
        for r in R:
            r.r = [x for x in r.r if x[0] != ev[0]] + [ev]
        for w in W:
            w.w = [ev]
            w.r = []
        return inst

    def dma(self, q, out, in_, reads=(), writes=(), **kw):
        R = self._regs(reads)
        W = self._regs(writes)
        lst, idx = self.dq[q]
        slot = lst[idx % len(lst)]
        self.dq[q][1] += 1
        evs = [(slot[0], slot[1])] if slot[1] > 0 else []
        for r in R:
            evs += r.w
        for w in W:
            evs += w.w
            evs += w.r
        self._wait(q, evs)
        slot[1] += 16
        ev = (slot[0], slot[1])
        self.e[q].dma_start(out=out, in_=in_, **kw).then_inc(self.sems[slot[0]], 16)
        self.ninst += 1
        for r in R:
            r.r = r.r + [ev]
        for w in W:
            w.w = [ev]
            w.r = []
        return ev

    def barrier(self):
        evs = []
        for en in self.ENG:
            k, v = self.cur[en]
            if v > 0:
                evs.append((k, v))
        for q in self.dq:
            for k, v in self.dq[q][0]:
                if v > 0:
                    evs.append((k, v))
        for en in self.ENG:
            self._wait(en, evs)


class _Scope:
    def __init__(self, kb):
        self.kb = kb

    def __enter__(self):
        es = ExitStack()
        self.kb.scopes.append(es)
        return es

    def __exit__(self, *a):
        self.kb.barrier()
        es = self.kb.scopes.pop()
        es.close()
        return False


def host_consts():
    c = {}
    c["cst_ident"] = np.eye(128, dtype=np.float32)
    blk = np.zeros((128, 128), np.float32)
    blk[:64, :64] = 1.0
    blk[64:, 64:] = 1.0
    c["cst_blk64"] = blk
    c["cst_ones"] = np.ones((128, 128), np.float32)
    rot = np.zeros((128, 128), np.float32)
    for i in range(64):
        rot[2 * i + 1, 2 * i] = -1.0
        rot[2 * i, 2 * i + 1] = 1.0
    c["cst_rot"] = rot
    rows = T // 64
    t_row = np.repeat(np.arange(rows, dtype=np.float32), 64)
    t_col = np.tile(np.arange(64, dtype=np.float32), rows)
    inv_freq = (10000.0 ** (-np.arange(0, 32, 2, dtype=np.float32) / 32)).astype(np.float32)
    ang = np.concatenate([t_row[:, None] * inv_freq, t_col[:, None] * inv_freq], axis=-1)
    cos = np.cos(ang).astype(np.float32)
    sin = np.sin(ang).astype(np.float32)
    pidx = (np.arange(128) % 64) // 2
    c["cst_cos"] = np.ascontiguousarray(cos[:, pidx].T)
    c["cst_sin"] = np.ascontiguousarray(sin[:, pidx].T)
    s_ = np.arange(128)[:, None]
    l_ = np.arange(128)[None, :]
    c["cst_triu"] = (l_ >= s_).astype(np.float32)
    c["cst_tril"] = (l_ <= s_).astype(np.float32)
    sel8 = np.zeros((8, 8, 128), np.float32)
    for e in range(8):
        sel8[e, e, :] = 1.0
    c["cst_sel8"] = sel8.reshape(8, 1024)
    return c


WEIGHT_NAMES = [
    "ev_ada_w", "ev_ada_b", "ev_norm1", "ev_norm2", "ev_w_in", "ev_q_gain", "ev_k_gain", "ev_dw_w", "ev_dw_b",
    "ev_ln_g", "ev_ln_b", "ev_w_o", "ev_ff_w1", "ev_ff_w3", "ev_ff_w2",
    "od_ada_w", "od_ada_b", "od_norm1", "od_norm2", "od_w_in", "od_conv_w", "od_conv_b", "od_a_log_f", "od_a_log_b",
    "od_dt_bias_f", "od_dt_bias_b", "od_d_skip", "od_gnorm", "od_w_out", "od_router", "od_ex_w1", "od_ex_w3",
    "od_ex_w2", "final_norm",
]


class Prog:
    def __init__(self, shapes, debug=()):
        self.nc = bass.Bass("TRN2", target_bir_lowering=False)
        self.kb = KB(self.nc)
        self.debug = set(debug)
        kb = self.kb
        self.inp = {}
        for name, shp in shapes.items():
            self.inp[name] = kb.dram(name, shp, F32, "ExternalInput")
        self.out = kb.dram("out", [T, D], F32, "ExternalOutput", nreg=T // 128)
        self.dbg = {}

    def dbg_out(self, name, shape, dt=F32):
        kind = "ExternalOutput" if name in self.debug else "Internal"
        b = self.kb.dram("dbg_" + name if kind == "ExternalOutput" else "scr_" + name, shape, dt, kind)
        return b

    def load_consts(self):
        kb, nc = self.kb, self.nc
        self.ident = kb.sb("ident", [128, 128], F32)
        kb.dma("sp", self.ident[:], self.inp["cst_ident"][:], writes=[self.ident])
        self.blk64 = kb.sb("blk64", [128, 128], BF16)
        kb.dma("pool", self.blk64[:], self.inp["cst_blk64"][:], writes=[self.blk64])
        self.ones_bf = kb.sb("ones_bf", [128, 128], BF16)
        kb.dma("pool", self.ones_bf[:], self.inp["cst_ones"][:], writes=[self.ones_bf])
        self.rot = kb.sb("rot", [128, 128], BF16)
        kb.dma("pool", self.rot[:], self.inp["cst_rot"][:], writes=[self.rot])
        self.PS = [kb.ps(f"psb{i}", [128, 512], F32) for i in range(8)]
        self.colT_st = kb.sb("colT_st", [48, 128], F32)
        self.eps_t = kb.sb("eps_t", [128, 1], F32)
        kb.op("dve", lambda: nc.vector.memset(self.eps_t[:], EPS), writes=[self.eps_t])

    def col_load(self, dst, src_row_ap, n):
        self.kb.dma("sp", dst, src_row_ap.rearrange("(c p) -> p c", p=128), writes=[], allow_slow_non_contiguous=True)

    def colT(self, dst, src2d, n, ps=None, wbuf=None):
        kb, nc = self.kb, self.nc
        st_ = self.colT_st
        kb.dma("sp", st_[0:n, :], src2d, writes=[st_])
        ps = self.PS[7] if ps is None else ps
        kb.op("pe", lambda: nc.tensor.transpose(ps[:, 0:n], st_[0:n, :], self.ident[0:n, 0:n]), reads=[st_, self.ident], writes=[ps])
        kb.op("dve", lambda: nc.vector.tensor_copy(out=dst, in_=ps[:, 0:n]), reads=[ps], writes=[wbuf])


    def xtile_src(self, src, t):
        ctx_ap, lat_ap = src[0], src[1]
        if t < 2:
            return ctx_ap[t * 128:(t + 1) * 128, :]
        return lat_ap[(t - 2) * 128:(t - 1) * 128, :]

    def norm_tile(self, xt, A, S, hnT, col0, pst, tmp, router=None):
        kb, nc = self.kb, self.nc
        junk, ss, xn = tmp
        kb.op("act", lambda: nc.scalar.activation(out=junk[:], in_=xt[:], func=AF.Square, accum_out=ss[:, 0:1]),
              reads=[xt], writes=[junk, ss])
        kb.op("act", lambda: nc.scalar.activation(out=ss[:, 1:2], in_=ss[:, 0:1], func=AF.Sqrt, scale=1.0 / D,
                                                  bias=self.eps_t[:, 0:1]), reads=[ss, self.eps_t], writes=[ss])
        kb.op("dve", lambda: nc.vector.reciprocal(out=ss[:, 2:3], in_=ss[:, 1:2]), reads=[ss], writes=[ss])
        kb.op("dve", lambda: nc.vector.tensor_scalar(out=xn[:], in0=xt[:], scalar1=ss[:, 2:3], scalar2=None,
                                                     op0=ALU.mult), reads=[xt, ss], writes=[xn])
        for half in range(2):
            ps = pst[half]
            for jj in range(4):
                j = half * 4 + jj
                kb.op("pe", lambda: nc.tensor.transpose(ps[:, jj * 128:(jj + 1) * 128], xn[:, j * 128:(j + 1) * 128],
                                                        self.ident[:]),
                      reads=[xn, self.ident], writes=[ps], signal=(jj == 3))
            for jj in range(4):
                j = half * 4 + jj
                kb.op("act", lambda: nc.scalar.activation(out=hnT[:, j, col0:col0 + 128],
                                                          in_=ps[:, jj * 128:(jj + 1) * 128], func=AF.Identity,
                                                          scale=A[:, j:j + 1], bias=S[:, j:j + 1]),
                      reads=[ps], writes=[hnT])
                if router is not None:
                    wr, hf, plg = router
                    kb.op("act", lambda: nc.scalar.activation(out=hf[:, j, :], in_=ps[:, jj * 128:(jj + 1) * 128], func=AF.Identity,
                                                              scale=A[:, j:j + 1], bias=S[:, j:j + 1]), reads=[ps], writes=[hf])
            if router is not None and half == 1:
                wr, hf, plg = router
                for j in range(8):
                    kb.op("pe", lambda: nc.tensor.matmul(plg[:, 0:32], lhsT=hf[:, j, :], rhs=wr[:, j, :], start=(j == 0),
                                                         stop=(j == 7)), reads=[hf, wr], writes=[plg], signal=(j == 7))

    def adaln(self, pre, norm1, norm2):
        kb, nc = self.kb, self.nc
        ada_w = self.inp[pre + "_ada_w"]
        ada_b = self.inp[pre + "_ada_b"]
        res = {}
        modT = kb.sb(pre + "modT", [128, 2, 48], F32)
        gbc = kb.sb(pre + "gbc", [128, 2, 2, 1024], F32)
        A1 = kb.sb(pre + "A1", [128, 2, 8], F32)
        A2 = kb.sb(pre + "A2", [128, 2, 8], F32)
        with kb.scope():
            cT = kb.sb("cT", [128, 2, 8], F32)
            self.colT(cT[:, 0, :], self.inp["c"][0, :].rearrange("(c p) -> c p", p=128), 8, ps=self.PS[5], wbuf=cT)
            self.colT(cT[:, 1, :], self.inp["c_ctx"][0, :].rearrange("(c p) -> c p", p=128), 8, ps=self.PS[6], wbuf=cT)
            sc = kb.sb("sc", [128, 2, 8], BF16)
            kb.op("act", lambda: nc.scalar.activation(out=sc[:], in_=cT[:], func=AF.Silu), reads=[cT], writes=[sc])
            scbc = kb.sb("scbc", [128, 2, 8, 128], BF16)
            for w in range(2):
                for kc in range(8):
                    kb.op("dve", lambda: nc.vector.tensor_copy(out=scbc[:, w, kc, :],
                                                               in_=sc[:, w, kc:kc + 1].to_broadcast([128, 128])),
                          reads=[sc], writes=[scbc])
            scr = kb.sb("scr", [128, 8, 2], BF16)
            for w in range(2):
                kb.op("dve", lambda: nc.vector.tensor_copy(out=scr[:, :, w], in_=sc[:, w, :]), reads=[sc], writes=[scr])
            bT = kb.sb("bT", [128, 48], F32)
            self.colT(bT[:], ada_b[0, :].rearrange("(c p) -> c p", p=128), 48, ps=self.PS[7], wbuf=bT)
            bbc = kb.sb("bbc", [128, 2, 1024], F32)
            for gi, c0 in enumerate((2048, 5120)):
                kb.dma("sp", bbc[:, gi, :], ada_b[0:1, c0:c0 + 1024].to_broadcast([128, 1024]), writes=[bbc])
            nT = kb.sb("nT", [128, 2, 8], F32)
            self.colT(nT[:, 0, :], norm1[0, :].rearrange("(c p) -> c p", p=128), 8, ps=self.PS[5], wbuf=nT)
            self.colT(nT[:, 1, :], norm2[0, :].rearrange("(c p) -> c p", p=128), 8, ps=self.PS[6], wbuf=nT)
            wv = ada_w.t[0].rearrange("(kc p) n -> p kc n", p=128)
            wbuf = [kb.sb(f"adaw{i}", [128, 8, 1024], BF16) for i in range(2)]
            pm = self.PS[0]
            for piece in range(6):
                wb = wbuf[piece % 2]
                for h in range(2):
                    kb.dma("pool", wb[:, :, h * 512:(h + 1) * 512], wv[:, :, piece * 1024 + h * 512:piece * 1024 + (h + 1) * 512],
                           writes=[wb])
                for fc in range(8):
                    j = piece * 8 + fc
                    for kc in range(8):
                        kb.op("pe", lambda: nc.tensor.matmul(pm[:, j * 2:j * 2 + 2], lhsT=wb[:, kc, fc * 128:(fc + 1) * 128],
                                                             rhs=scr[:, kc, :], start=(kc == 0), stop=(kc == 7)),
                              reads=[wb, scr], writes=[pm], signal=(kc == 7))
                if piece in (2, 5):
                    gi = 0 if piece == 2 else 1
                    for w in range(2):
                        for h in range(2):
                            pg = self.PS[1 + (w * 2 + h) % 4]
                            for kc in range(8):
                                kb.op("pe", lambda: nc.tensor.matmul(pg[:, :], lhsT=scbc[:, w, kc, :],
                                                                     rhs=wb[:, kc, h * 512:(h + 1) * 512],
                                                                     start=(kc == 0), stop=(kc == 7)),
                                      reads=[wb, scbc], writes=[pg], signal=(kc == 7))
                            kb.op("dve", lambda: nc.vector.tensor_tensor(out=gbc[:, w, gi, h * 512:(h + 1) * 512], in0=pg[:, :],
                                                                         in1=bbc[:, gi, h * 512:(h + 1) * 512], op=ALU.add),
                                  reads=[pg, bbc], writes=[gbc])
            for w in range(2):
                kb.op("dve", lambda: nc.vector.tensor_tensor(
                    out=modT[:, w, :], in0=pm[:, 0:96].rearrange("p (j w) -> p j w", w=2)[:, :, w], in1=bT[:], op=ALU.add),
                    reads=[pm, bT], writes=[modT])
                kb.op("dve", lambda: nc.vector.scalar_tensor_tensor(out=A1[:, w, :], in0=modT[:, w, 8:16], scalar=1.0,
                                                                    in1=nT[:, 0, :], op0=ALU.add, op1=ALU.mult),
                      reads=[modT, nT], writes=[A1])
                kb.op("dve", lambda: nc.vector.scalar_tensor_tensor(out=A2[:, w, :], in0=modT[:, w, 32:40], scalar=1.0,
                                                                    in1=nT[:, 1, :], op0=ALU.add, op1=ALU.mult),
                      reads=[modT, nT], writes=[A2])
        res["A1"] = A1
        res["A2"] = A2
        res["modT"] = modT
        res["gbc"] = gbc
        return res

    def even_layer(self, src, res_out):
        kb, nc = self.kb, self.nc
        inp = self.inp
        PS = self.PS
        with kb.scope():
            ada = self.adaln("ev", inp["ev_norm1"], inp["ev_norm2"])
            A1, A2, modT, gbc = ada["A1"], ada["A2"], ada["modT"], ada["gbc"]
            res1 = self.dbg_out("res1", [TT, D])
            res1.regs = [Reg() for _ in range(NT)]
            hn2T = kb.sb("hn2T", [128, 8, TT], BF16, nreg=NT)
            with kb.scope():
                qT = kb.sb("qT", [128, 4, TT], BF16, nreg=5)
                kT = kb.sb("kT", [128, 2, TT], BF16, nreg=5)
                kf = [kb.sb(f"kf{i}", [128, 512], F32) for i in range(2)]
                vaug = kb.sb("vaug", [128, NT, 2, 128], BF16, nreg=NT)
                glu = kb.sb("glu", [128, 4, TT], BF16, nreg=5)
                kb.op("pool", lambda: nc.gpsimd.memset(vaug[:], 1.0), writes=[vaug] + vaug.regs)
                with kb.scope():
                    w_in = kb.sb("w_in", [128, 8, 1792], BF16)
                    wv = inp["ev_w_in"].t[0].rearrange("(kc p) n -> p kc n", p=128)
                    for i in range(4):
                        kb.dma("pool", w_in[:, :, i * 448:(i + 1) * 448], wv[:, :, i * 448:(i + 1) * 448], writes=[w_in])
                    cos = kb.sb("cos", [128, T], F32)
                    sin = kb.sb("sin", [128, T], F32)
                    kb.dma("sp", cos[:], inp["cst_cos"][:], writes=[cos])
                    kb.dma("sp", sin[:], inp["cst_sin"][:], writes=[sin])
                    gain = kb.sb("gain", [128, 2], F32)
                    for hh in range(2):
                        kb.dma("sp", gain[hh * 64:(hh + 1) * 64, 0:1], inp["ev_q_gain"][0, :].rearrange("(p o) -> p o", o=1),
                               writes=[gain], allow_slow_non_contiguous=True)
                        kb.dma("sp", gain[hh * 64:(hh + 1) * 64, 1:2], inp["ev_k_gain"][0, :].rearrange("(p o) -> p o", o=1),
                               writes=[gain], allow_slow_non_contiguous=True)
                    xt = [kb.sb(f"xt{i}", [128, D], F32) for i in range(2)]
                    junk = kb.sb("junk", [128, D], BF16)
                    xn = [kb.sb(f"xn{i}", [128, D], F32) for i in range(1)] * 2
                    ss = [kb.sb(f"ss{i}", [128, 4], F32) for i in range(2)]
                    hnT = [kb.sb(f"hnT{i}", [128, 8, 512], BF16) for i in range(2)]
                    sq = [kb.sb(f"sq{i}", [128, 512], BF16) for i in range(2)]
                    rr = [kb.sb(f"rr{i}", [128, 512], F32) for i in range(2)]
                    qn = [kb.sb(f"qn{i}", [128, 512], F32) for i in range(2)]
                    qb = [kb.sb(f"qb{i}", [128, 512], BF16) for i in range(2)]
                    t1 = [kb.sb(f"t1{i}", [128, 512], F32) for i in range(1)] * 2
                    sg = [kb.sb(f"sg{i}", [128, 512], F32) for i in range(1)] * 2
                    tcount = 0
                    it = 0
                    for bi, (s0, n) in enumerate(BLOCKS):
                        hb = hnT[bi % 2]
                        w = 1 if bi == 0 else 0
                        for tl in range(n // 128):
                            t = s0 // 128 + tl
                            x_ = xt[tcount % 2]
                            kb.dma("sp", x_[:], self.xtile_src(src, t), writes=[x_])
                            self.norm_tile(x_, A1[:, w, :], modT[:, w, 0:8], hb, tl * 128, (PS[0], PS[1]),
                                           (junk, ss[tcount % 2], xn[tcount % 2]))
                            pv = PS[2]
                            for kc in range(8):
                                kb.op("pe", lambda: nc.tensor.matmul(pv[:, 0:128], lhsT=hb[:, kc, tl * 128:(tl + 1) * 128],
                                                                     rhs=w_in[:, kc, 640:768], start=(kc == 0), stop=(kc == 7)),
                                      reads=[hb, w_in], writes=[pv], signal=(kc == 7))
                            kb.op("dve", lambda: nc.vector.tensor_copy(
                                out=vaug[:, t, :, 0:64], in_=pv[:, 0:128].rearrange("p (g d) -> p g d", g=2)),
                                reads=[pv], writes=[vaug.regs[t]])
                            tcount += 1
                        for j in range(5):
                            pq = PS[3 + it % 2]
                            pss = PS[5]
                            ppq = PS[6]
                            i2 = it % 2
                            it += 1
                            for kc in range(8):
                                kb.op("pe", lambda: nc.tensor.matmul(pq[:, 0:n], lhsT=w_in[:, kc, j * 128:(j + 1) * 128],
                                                                     rhs=hb[:, kc, 0:n], start=(kc == 0), stop=(kc == 7)),
                                      reads=[hb, w_in], writes=[pq], signal=(kc == 7))
                            kb.op("act", lambda: nc.scalar.activation(out=sq[i2][:, 0:n], in_=pq[:, 0:n], func=AF.Square),
                                  reads=[pq], writes=[sq[i2]])
                            kb.op("pe", lambda: nc.tensor.matmul(pss[:, 0:n], lhsT=self.blk64[:], rhs=sq[i2][:, 0:n],
                                                                 start=True, stop=True),
                                  reads=[sq[i2], self.blk64], writes=[pss])
                            kb.op("act", lambda: nc.scalar.activation(out=rr[i2][:, 0:n], in_=pss[:, 0:n], func=AF.Sqrt,
                                                                      scale=1.0 / 64, bias=self.eps_t[:, 0:1]),
                                  reads=[pss, self.eps_t], writes=[rr[i2]])
                            kb.op("dve", lambda: nc.vector.reciprocal(out=rr[i2][:, 0:n], in_=rr[i2][:, 0:n]),
                                  reads=[rr[i2]], writes=[rr[i2]])
                            gcol = gain[:, 0:1] if j < 4 else gain[:, 1:2]
                            if j < 4:
                                dst = qT[:, j, s0:s0 + n]
                                dreg = qT.regs[bi]
                            else:
                                dst = kf[bi % 2][:, 0:n]
                                dreg = kf[bi % 2]
                            if bi == 0:
                                kb.op("dve", lambda: nc.vector.scalar_tensor_tensor(out=dst, in0=pq[:, 0:n], scalar=gcol,
                                                                                    in1=rr[i2][:, 0:n], op0=ALU.mult,
                                                                                    op1=ALU.mult),
                                      reads=[pq, gain, rr[i2]], writes=[dreg])
                            else:
                                l0 = s0 - TC
                                kb.op("dve", lambda: nc.vector.scalar_tensor_tensor(out=qn[i2][:, 0:n], in0=pq[:, 0:n],
                                                                                    scalar=gcol, in1=rr[i2][:, 0:n],
                                                                                    op0=ALU.mult, op1=ALU.mult),
                                      reads=[pq, gain, rr[i2]], writes=[qn[i2]])
                                kb.op("act", lambda: nc.scalar.activation(out=qb[i2][:, 0:n], in_=qn[i2][:, 0:n], func=AF.Copy),
                                      reads=[qn[i2]], writes=[qb[i2]])
                                kb.op("pe", lambda: nc.tensor.matmul(ppq[:, 0:n], lhsT=self.rot[:], rhs=qb[i2][:, 0:n],
                                                                     start=True, stop=True),
                                      reads=[qb[i2], self.rot], writes=[ppq])
                                kb.op("pool", lambda: nc.gpsimd.tensor_tensor(out=t1[i2][:, 0:n], in0=qn[i2][:, 0:n],
                                                                              in1=cos[:, l0:l0 + n], op=ALU.mult),
                                      reads=[qn[i2], cos], writes=[t1[i2]])
                                kb.op("dve", lambda: nc.vector.tensor_tensor(out=qn[i2][:, 0:n], in0=ppq[:, 0:n],
                                                                             in1=sin[:, l0:l0 + n], op=ALU.mult),
                                      reads=[ppq, sin, qn[i2]], writes=[qn[i2]])
                                kb.op("dve", lambda: nc.vector.tensor_tensor(out=dst, in0=qn[i2][:, 0:n], in1=t1[i2][:, 0:n],
                                                                             op=ALU.add),
                                      reads=[qn[i2], t1[i2]], writes=[dreg])
                        kfb = kf[bi % 2]
                        for g_ in range(2):
                            for hf in range(2):
                                kb.op("act", lambda: nc.scalar.activation(out=kT[hf * 64:(hf + 1) * 64, g_, s0:s0 + n],
                                                                          in_=kfb[g_ * 64:(g_ + 1) * 64, 0:n], func=AF.Copy),
                                      reads=[kfb], writes=[kT.regs[bi]])
                        for j in range(4):
                            pa = PS[3 + it % 2]
                            pg = PS[6 + it % 2]
                            i2 = it % 2
                            it += 1
                            for kc in range(8):
                                kb.op("pe", lambda: nc.tensor.matmul(pa[:, 0:n], lhsT=w_in[:, kc, 768 + j * 128:768 + (j + 1) * 128],
                                                                     rhs=hb[:, kc, 0:n], start=(kc == 0), stop=(kc == 7)),
                                      reads=[hb, w_in], writes=[pa], signal=(kc == 7))
                            for kc in range(8):
                                kb.op("pe", lambda: nc.tensor.matmul(pg[:, 0:n], lhsT=w_in[:, kc, 1280 + j * 128:1280 + (j + 1) * 128],
                                                                     rhs=hb[:, kc, 0:n], start=(kc == 0), stop=(kc == 7)),
                                      reads=[hb, w_in], writes=[pg], signal=(kc == 7))
                            kb.op("act", lambda: nc.scalar.activation(out=sg[i2][:, 0:n], in_=pg[:, 0:n], func=AF.Sigmoid),
                                  reads=[pg], writes=[sg[i2]])
                            kb.op("dve", lambda: nc.vector.tensor_tensor(out=glu[:, j, s0:s0 + n], in0=pa[:, 0:n],
                                                                         in1=sg[i2][:, 0:n], op=ALU.mult),
                                  reads=[pa, sg[i2]], writes=[glu.regs[bi]])
                if "ev_q" in self.debug:
                    dq = self.dbg_out("ev_q", [128, 4, TT], BF16)
                    kb.dma("sp", dq[:], qT[:], reads=qT.regs, writes=[dq])
                    dk = self.dbg_out("ev_k", [128, 2, TT], BF16)
                    kb.dma("sp", dk[:], kT[:], reads=kT.regs, writes=[dk])
                    dg = self.dbg_out("ev_glu", [128, 4, TT], BF16)
                    kb.dma("sp", dg[:], glu[:], reads=glu.regs, writes=[dg])
                    dv = self.dbg_out("ev_v", [128, NT, 2, 128], BF16)
                    kb.dma("sp", dv[:], vaug[:], reads=vaug.regs, writes=[dv])
                mix = kb.sb("mix", [128, 8, TT], BF16, nreg=5)
                with kb.scope():
                    cw = kb.sb("cw", [128, 4, 31], F32)
                    for j_ in range(4):
                        self.colT(cw[:, j_, :], inp["ev_dw_w"][0][:, j_ * 128:(j_ + 1) * 128], 31, ps=self.PS[4 + j_ % 2], wbuf=cw)
                    cb = kb.sb("cb", [128, 4, 3], F32)
                    for i, nm in enumerate(("ev_dw_b", "ev_ln_g", "ev_ln_b")):
                        self.colT(cb[:, :, i], inp[nm][0, :].rearrange("(c p) -> c p", p=128), 4, ps=self.PS[4 + i % 2], wbuf=cb)
                    cv = kb.sb("cv", [128, 4, TT], F32, nreg=4)
                    conv_ops = []

                    def mk_first(j, s0, n):
                        return lambda: kb.op("dve", lambda: nc.vector.tensor_scalar(
                            out=cv[:, j, s0:s0 + n], in0=glu[:, j, s0:s0 + n], scalar1=cw[:, j, 15:16], scalar2=cb[:, j, 0:1],
                            op0=ALU.mult, op1=ALU.add), reads=glu.regs + [cw, cb], writes=[cv.regs[j]])

                    def mk_tap(j, s0, lo, hi, o, k):
                        return lambda: kb.op("dve", lambda: nc.vector.scalar_tensor_tensor(
                            out=cv[:, j, s0 + lo:s0 + hi], in0=glu[:, j, s0 + lo + o:s0 + hi + o], scalar=cw[:, j, k:k + 1],
                            in1=cv[:, j, s0 + lo:s0 + hi], op0=ALU.mult, op1=ALU.add),
                            reads=glu.regs + [cw, cv.regs[j]], writes=[cv.regs[j]])

                    for j in range(4):
                        for (s0, n) in ((0, TC), (TC, T)):
                            conv_ops.append(mk_first(j, s0, n))
                            for k in range(31):
                                o = k - 15
                                if o == 0:
                                    continue
                                conv_ops.append(mk_tap(j, s0, max(0, -o), min(n, n - o), o, k))
                    conv_ops.reverse()
                    self._attention(qT, kT, vaug, mix, conv_ops)
                    while conv_ops:
                        conv_ops.pop()()
                    xb = [kb.sb(f"lnxb{i}", [128, 512], BF16) for i in range(4)]
                    x2 = [kb.sb(f"lnx2{i}", [128, 512], BF16) for i in range(4)]
                    mv = kb.sb("lnmv", [128, 3, 512], F32)
                    tmpc = [kb.sb(f"lntmp{i}", [128, 512], F32) for i in range(1)] * 2
                    for bi, (s0, n) in enumerate(BLOCKS):
                        pm, p2 = PS[0 + 2 * (bi % 2)], PS[1 + 2 * (bi % 2)]
                        for j in range(4):
                            kb.op("act", lambda: nc.scalar.activation(out=xb[j][:, 0:n], in_=cv[:, j, s0:s0 + n], func=AF.Copy),
                                  reads=[cv.regs[j]], writes=[xb[j]])
                            kb.op("act", lambda: nc.scalar.activation(out=x2[j][:, 0:n], in_=cv[:, j, s0:s0 + n], func=AF.Square),
                                  reads=[cv.regs[j]], writes=[x2[j]])
                        for j in range(4):
                            kb.op("pe", lambda: nc.tensor.matmul(pm[:, 0:n], lhsT=self.ones_bf[:], rhs=xb[j][:, 0:n],
                                                                 start=(j == 0), stop=(j == 3)),
                                  reads=[xb[j], self.ones_bf], writes=[pm], signal=(j == 3))
                        for j in range(4):
                            kb.op("pe", lambda: nc.tensor.matmul(p2[:, 0:n], lhsT=self.ones_bf[:], rhs=x2[j][:, 0:n],
                                                                 start=(j == 0), stop=(j == 3)),
                                  reads=[x2[j], self.ones_bf], writes=[p2], signal=(j == 3))
                        kb.op("act", lambda: nc.scalar.activation(out=mv[:, 0, 0:n], in_=pm[:, 0:n], func=AF.Copy, scale=1.0 / 512),
                              reads=[pm], writes=[mv])
                        kb.op("dve", lambda: nc.vector.tensor_tensor(out=mv[:, 1, 0:n], in0=mv[:, 0, 0:n], in1=mv[:, 0, 0:n],
                                                                     op=ALU.mult), reads=[mv], writes=[mv])
                        kb.op("dve", lambda: nc.vector.scalar_tensor_tensor(out=mv[:, 1, 0:n], in0=p2[:, 0:n], scalar=1.0 / 512,
                                                                            in1=mv[:, 1, 0:n], op0=ALU.mult, op1=ALU.subtract),
                              reads=[p2, mv], writes=[mv])
                        kb.op("act", lambda: nc.scalar.activation(out=mv[:, 2, 0:n], in_=mv[:, 1, 0:n], func=AF.Sqrt,
                                                                  bias=self.eps_t[:, 0:1]), reads=[mv, self.eps_t], writes=[mv])
                        kb.op("dve", lambda: nc.vector.reciprocal(out=mv[:, 2, 0:n], in_=mv[:, 2, 0:n]), reads=[mv], writes=[mv])
                        for j in range(4):
                            tc_ = tmpc[j % 2]
                            kb.op("dve", lambda: nc.vector.tensor_tensor(out=tc_[:, 0:n], in0=cv[:, j, s0:s0 + n], in1=mv[:, 0, 0:n],
                                                                         op=ALU.subtract), reads=[cv.regs[j], mv], writes=[tc_])
                            kb.op("dve", lambda: nc.vector.tensor_tensor(out=tc_[:, 0:n], in0=tc_[:, 0:n], in1=mv[:, 2, 0:n],
                                                                         op=ALU.mult), reads=[tc_, mv], writes=[tc_])
                            kb.op("act", lambda: nc.scalar.activation(out=mix[:, 4 + j, s0:s0 + n], in_=tc_[:, 0:n], func=AF.Silu,
                                                                      scale=cb[:, j, 1:2], bias=cb[:, j, 2:3]),
                                  reads=[tc_, cb], writes=[mix.regs[bi]])
                with kb.scope():
                    w_o = kb.sb("w_o", [128, 8, 1024], BF16)
                    wv = inp["ev_w_o"].t[0].rearrange("(kc p) n -> p kc n", p=128)
                    for i in range(2):
                        kb.dma("pool", w_o[:, :, i * 512:(i + 1) * 512], wv[:, :, i * 512:(i + 1) * 512], writes=[w_o])
                    self.proj_residual(src, mix, w_o, 8, gbc, 0, None, res1, A2, modT[:, :, 24:32], hn2T)
            with kb.scope():
                yacc = kb.sb("yacc", [128, NT, D], F32, nreg=NT)
                w1v = inp["ev_ff_w1"].t[0].rearrange("(kc p) n -> p kc n", p=128)
                w3v = inp["ev_ff_w3"].t[0].rearrange("(kc p) n -> p kc n", p=128)
                w2v = inp["ev_ff_w2"].t[0].rearrange("(c p) n -> p c n", p=128)
                fblocks = [(i * 512, 512) for i in range(5)] + [(2560, 256)]
                self.ffn(hn2T, BLOCKS, [(w1v, w3v, w2v, f0, fw, None) for (f0, fw) in fblocks], yacc)
                self.final_residual(res1, yacc, gbc, 1, res_out, list(range(NT)))


    def _attention(self, qT, kT, vaug, mix, conv_ops):
        kb, nc = self.kb, self.nc
        PS = self.PS
        pT = [kb.sb(f"pT{i}", [128, 512], BF16) for i in range(2)]
        rden = [kb.sb(f"rden{i}", [64, 512], F32) for i in range(1)] * 2
        on = [kb.sb(f"on{i}", [64, 512], F32) for i in range(1)] * 2
        cnt = 0
        hc = 0
        for bi, (s0, n) in enumerate(BLOCKS):
            ktiles = range(0, 2) if bi == 0 else range(0, NT)
            for h in range(8):
                g = h // 4
                ch, half = h // 2, h % 2
                po = PS[6 + hc % 2]
                i2 = hc % 2
                hc += 1
                nk = len(ktiles)
                kts = list(ktiles)
                base = cnt
                cnt += nk

                def qk(i):
                    pss_ = PS[(base + i) % 4]
                    kt_ = kts[i]
                    kb.op("pe", lambda: nc.tensor.matmul(
                        pss_[:, 0:n], lhsT=kT[half * 64:(half + 1) * 64, g, kt_ * 128:(kt_ + 1) * 128],
                        rhs=qT[half * 64:(half + 1) * 64, ch, s0:s0 + n], start=True, stop=True),
                        reads=kT.regs + [qT.regs[bi]], writes=[pss_])

                qk(0)
                if nk > 1:
                    qk(1)
                for ki in range(nk):
                    pss = PS[(base + ki) % 4]
                    pt_ = pT[(base + ki) % 2]
                    kt = kts[ki]
                    if conv_ops and (base + ki) % 2 == 0:
                        conv_ops.pop()()
                    kb.op("act", lambda: nc.scalar.activation(out=pt_[:, 0:n], in_=pss[:, 0:n], func=AF.Exp, scale=0.125),
                          reads=[pss], writes=[pt_])
                    if ki + 2 < nk:
                        qk(ki + 2)
                    kb.op("pe", lambda: nc.tensor.matmul(po[:, 0:n], lhsT=vaug[:, kt, g, :], rhs=pt_[:, 0:n],
                                                         start=(ki == 0), stop=(ki == nk - 1)),
                          reads=[pt_, vaug.regs[kt]], writes=[po])
                kb.op("act", lambda: nc.scalar.activation(out=rden[i2][:, 0:n], in_=po[64:128, 0:n], func=AF.Copy),
                      reads=[po], writes=[rden[i2]])
                kb.op("dve", lambda: nc.vector.reciprocal(out=rden[i2][:, 0:n], in_=rden[i2][:, 0:n]),
                      reads=[rden[i2]], writes=[rden[i2]])
                if half == 0:
                    kb.op("dve", lambda: nc.vector.tensor_tensor(out=mix[0:64, ch, s0:s0 + n], in0=po[0:64, 0:n],
                                                                 in1=rden[i2][:, 0:n], op=ALU.mult),
                          reads=[po, rden[i2]], writes=[mix.regs[bi]])
                else:
                    kb.op("dve", lambda: nc.vector.tensor_tensor(out=on[i2][:, 0:n], in0=po[0:64, 0:n],
                                                                 in1=rden[i2][:, 0:n], op=ALU.mult),
                          reads=[po, rden[i2]], writes=[on[i2]])
                    kb.op("act", lambda: nc.scalar.activation(out=mix[64:128, ch, s0:s0 + n], in_=on[i2][:, 0:n],
                                                              func=AF.Copy),
                          reads=[on[i2]], writes=[mix.regs[bi]])

    def proj_residual(self, src, actT, w_sb, nkc, gbc, gi, rowscale, res1, A2, S2, hn2T, tiles=None):
        kb, nc = self.kb, self.nc
        PS = self.PS
        xr = [kb.sb(f"pr_x{i}", [128, D], F32) for i in range(2)]
        tm = [kb.sb(f"pr_t{i}", [128, D], F32) for i in range(2)]
        junk = kb.sb("pr_junk", [128, D], F32)
        xn = [kb.sb(f"pr_xn{i}", [128, D], F32) for i in range(2)]
        ss = [kb.sb(f"pr_ss{i}", [128, 4], F32) for i in range(2)]
        tiles = list(range(NT)) if tiles is None else tiles
        for i, t in enumerate(tiles):
            w = 1 if t < 2 else 0
            i2 = i % 2
            x_ = xr[i2]
            kb.dma("sp", x_[:], self.xtile_src(src, t), writes=[x_])
            for h in range(2):
                po = PS[2 + 2 * i2 + h]
                for kc in range(nkc):
                    kb.op("pe", lambda: nc.tensor.matmul(po[:, :], lhsT=actT[:, kc, t * 128:(t + 1) * 128],
                                                         rhs=w_sb[:, kc, h * 512:(h + 1) * 512], start=(kc == 0),
                                                         stop=(kc == nkc - 1)),
                          reads=[actT] + actT.regs + [w_sb], writes=[po], signal=(kc == nkc - 1))
                if rowscale is not None:
                    kb.op("act", lambda: nc.scalar.activation(out=tm[i2][:, h * 512:(h + 1) * 512], in_=po[:, :], func=AF.Identity,
                                                              scale=rowscale[:, t:t + 1]),
                          reads=[po, rowscale], writes=[tm[i2]])
                    kb.op("dve", lambda: nc.vector.tensor_tensor(out=tm[i2][:, h * 512:(h + 1) * 512], in0=tm[i2][:, h * 512:(h + 1) * 512],
                                                                 in1=gbc[:, w, gi, h * 512:(h + 1) * 512], op=ALU.mult),
                          reads=[tm[i2], gbc], writes=[tm[i2]])
                else:
                    kb.op("dve", lambda: nc.vector.tensor_tensor(out=tm[i2][:, h * 512:(h + 1) * 512], in0=po[:, :],
                                                                 in1=gbc[:, w, gi, h * 512:(h + 1) * 512], op=ALU.mult),
                          reads=[po, gbc], writes=[tm[i2]])
            kb.op("pool", lambda: nc.gpsimd.tensor_tensor(out=x_[:], in0=x_[:], in1=tm[i2][:], op=ALU.add),
                  reads=[x_, tm[i2]], writes=[x_])
            kb.dma("sp", res1[t * 128:(t + 1) * 128, :], x_[:], reads=[x_], writes=[res1.regs[t]])
            self.norm_tile(x_, A2[:, w, :], S2[:, w, :], hn2T, t * 128, (PS[0], PS[1]), (junk, ss[i2], xn[i2]))

    def ffn(self, hnT, blocks, wblocks, yacc, gate_fn=None):
        kb, nc = self.kb, self.nc
        PS = self.PS
        w1b = [kb.sb(f"ffw1_{i}", [128, 8, 512], BF16) for i in range(2)]
        w3b = [kb.sb(f"ffw3_{i}", [128, 8, 512], BF16) for i in range(2)]
        w2b = [kb.sb(f"ffw2_{i}", [128, 4, D], BF16) for i in range(2)]
        sg = [kb.sb(f"ffsg{i}", [128, 512], F32) for i in range(2)]
        gg = [kb.sb(f"ffg{i}", [128, 4, 512], BF16) for i in range(2)]
        it = 0
        ib = 0
        iy = 0
        started = set()
        def issue_loads(wi_):
            w1v_, w3v_, w2v_, f0_, fw_, _g = wblocks[wi_]
            nfc_ = fw_ // 128
            kb.dma("pool", w1b[wi_ % 2][:, :, 0:fw_], w1v_[:, :, f0_:f0_ + fw_], writes=[w1b[wi_ % 2]])
            kb.dma("pool", w3b[wi_ % 2][:, :, 0:fw_], w3v_[:, :, f0_:f0_ + fw_], writes=[w3b[wi_ % 2]])
            kb.dma("pool", w2b[wi_ % 2][:, 0:nfc_, :], w2v_[:, f0_ // 128:f0_ // 128 + nfc_, :], writes=[w2b[wi_ % 2]])

        st = {"iy": 0, "pending": None}
        issue_loads(0)
        for wi, (w1v, w3v, w2v, f0, fw, gate) in enumerate(wblocks):
            b1, b3, b2 = w1b[wi % 2], w3b[wi % 2], w2b[wi % 2]
            nfc = fw // 128
            if st["pending"] is not None:
                st["pending"]()
                st["pending"] = None
            if wi + 1 < len(wblocks):
                issue_loads(wi + 1)
            gbcast = gate_fn(gate) if gate is not None else None
            for (s0, n) in blocks:
                g_ = gg[ib % 2]
                ib += 1
                for c in range(nfc):
                    p1 = PS[it % 2]
                    p3 = PS[2 + it % 2]
                    s_ = sg[it % 2]
                    it += 1
                    for kc in range(8):
                        kb.op("pe", lambda: nc.tensor.matmul(p1[:, 0:n], lhsT=b1[:, kc, c * 128:(c + 1) * 128], rhs=hnT[:, kc, s0:s0 + n],
                                                             start=(kc == 0), stop=(kc == 7)),
                              reads=[b1, hnT] + hnT.regs, writes=[p1], signal=(kc == 7))
                    for kc in range(8):
                        kb.op("pe", lambda: nc.tensor.matmul(p3[:, 0:n], lhsT=b3[:, kc, c * 128:(c + 1) * 128], rhs=hnT[:, kc, s0:s0 + n],
                                                             start=(kc == 0), stop=(kc == 7)),
                              reads=[b3, hnT] + hnT.regs, writes=[p3], signal=(kc == 7))
                    kb.op("act", lambda: nc.scalar.activation(out=s_[:, 0:n], in_=p1[:, 0:n], func=AF.Silu), reads=[p1], writes=[s_])
                    if gbcast is None:
                        kb.op("dve", lambda: nc.vector.tensor_tensor(out=g_[:, c, 0:n], in0=p3[:, 0:n], in1=s_[:, 0:n], op=ALU.mult),
                              reads=[p3, s_], writes=[g_])
                    else:
                        kb.op("dve", lambda: nc.vector.tensor_tensor(out=s_[:, 0:n], in0=p3[:, 0:n], in1=s_[:, 0:n], op=ALU.mult),
                              reads=[p3, s_], writes=[s_])
                        kb.op("pool", lambda: nc.gpsimd.tensor_tensor(out=g_[:, c, 0:n], in0=s_[:, 0:n], in1=gbcast[:, s0:s0 + n], op=ALU.mult),
                              reads=[s_, gbcast], writes=[g_])
                def py_stage(g_=g_, b2=b2, s0=s0, n=n, nfc=nfc):
                    for tl in range(n // 128):
                        t = s0 // 128 + tl
                        for h in range(2):
                            py = PS[4 + st["iy"] % 4]
                            st["iy"] += 1
                            for c in range(nfc):
                                kb.op("pe", lambda: nc.tensor.matmul(py[:, :], lhsT=g_[:, c, tl * 128:(tl + 1) * 128], rhs=b2[:, c, h * 512:(h + 1) * 512],
                                                                     start=(c == 0), stop=(c == nfc - 1)),
                                      reads=[g_, b2], writes=[py], signal=(c == nfc - 1))
                            if (t, h) not in started:
                                started.add((t, h))
                                kb.op("act", lambda: nc.scalar.activation(out=yacc[:, t, h * 512:(h + 1) * 512], in_=py[:, :], func=AF.Copy),
                                      reads=[py], writes=[yacc.regs[t]])
                            else:
                                kb.op("dve", lambda: nc.vector.tensor_tensor(out=yacc[:, t, h * 512:(h + 1) * 512], in0=py[:, :],
                                                                             in1=yacc[:, t, h * 512:(h + 1) * 512], op=ALU.add),
                                      reads=[py, yacc.regs[t]], writes=[yacc.regs[t]])

                if st["pending"] is not None:
                    st["pending"]()
                st["pending"] = py_stage
        if st["pending"] is not None:
            st["pending"]()
            st["pending"] = None

    def final_residual(self, res1, yacc, gbc, gi, res_out, tiles, final_norm=None, lat_only=False):
        kb, nc = self.kb, self.nc
        xr = [kb.sb(f"fr_x{i}", [128, D], F32) for i in range(2)]
        if final_norm is not None:
            fnb = kb.sb("fr_fn", [128, D], F32)
            kb.dma("sp", fnb[:], final_norm[0:1, :].to_broadcast([128, D]), writes=[fnb])
            junk = kb.sb("fr_junk", [128, D], F32)
            ss = [kb.sb(f"fr_ss{i}", [128, 4], F32) for i in range(2)]
        for i, t in enumerate(tiles):
            w = 1 if (t < 2 and not lat_only) else 0
            x_ = xr[i % 2]
            kb.dma("sp", x_[:], res1[t * 128:(t + 1) * 128, :], reads=[res1.regs[t]], writes=[x_])
            kb.op("dve", lambda: nc.vector.tensor_tensor(out=yacc[:, t, :], in0=yacc[:, t, :], in1=gbc[:, w, gi, :], op=ALU.mult),
                  reads=[yacc.regs[t], gbc], writes=[yacc.regs[t]])
            kb.op("pool", lambda: nc.gpsimd.tensor_tensor(out=x_[:], in0=x_[:], in1=yacc[:, t, :], op=ALU.add),
                  reads=[x_, yacc.regs[t]], writes=[x_])
            if final_norm is None:
                kb.dma("sp", res_out[t * 128:(t + 1) * 128, :], x_[:], reads=[x_], writes=[res_out.regs[t]])
            else:
                s_ = ss[i % 2]
                kb.op("act", lambda: nc.scalar.activation(out=junk[:], in_=x_[:], func=AF.Square, accum_out=s_[:, 0:1]),
                      reads=[x_], writes=[junk, s_])
                kb.op("act", lambda: nc.scalar.activation(out=s_[:, 1:2], in_=s_[:, 0:1], func=AF.Sqrt, scale=1.0 / D,
                                                          bias=self.eps_t[:, 0:1]), reads=[s_, self.eps_t], writes=[s_])
                kb.op("dve", lambda: nc.vector.reciprocal(out=s_[:, 2:3], in_=s_[:, 1:2]), reads=[s_], writes=[s_])
                kb.op("dve", lambda: nc.vector.scalar_tensor_tensor(out=x_[:], in0=x_[:], scalar=s_[:, 2:3], in1=fnb[:],
                                                                    op0=ALU.mult, op1=ALU.mult),
                      reads=[x_, s_, fnb], writes=[x_])
                lt = t if lat_only else t - 2
                kb.dma("sp", res_out[lt * 128:(lt + 1) * 128, :], x_[:], reads=[x_], writes=[res_out.regs[lt]])

    def odd_layer(self, src, final_out):
        kb, nc = self.kb, self.nc
        inp = self.inp
        PS = self.PS
        NH = 32
        with kb.scope():
            ada = self.adaln("od", inp["od_norm1"], inp["od_norm2"])
            A1, A2, modT, gbc = ada["A1"], ada["A2"], ada["modT"], ada["gbc"]
            hn2T = kb.sb("ohn2T", [128, 8, T], BF16, nreg=16)
            gates = kb.sb("gates", [128, 16, 8], F32, nreg=16)
            xmid = self.dbg_out("od_xmid", [T, D])
            xmid.regs = [Reg() for _ in range(16)]
            zsT = self.dbg_out("zsT", [16, 128, T], BF16)
            xsT = self.dbg_out("xsT", [16, 128, TT], BF16)
            BT = self.dbg_out("BT", [4, 128, TT], BF16)
            CT = self.dbg_out("CT", [4, 128, TT], BF16)
            xs_tok = self.dbg_out("xs_tok", [NT, 128, 2048], BF16)
            B_tok = self.dbg_out("B_tok", [NT, 128, 512], BF16)
            Sst = self.dbg_out("Sst", [2, NT, 128, 2048], BF16)
            for b_ in (Sst,):
                b_.regs = [Reg() for _ in range(2 * NT)]
            with kb.scope():
                dt_sb = kb.sb("dt_sb", [128, NT, 64], F32)
                a_sb = kb.sb("a_sb", [128, NT, 64], F32)
                with kb.scope():
                    hnT = kb.sb("ohnT", [128, 8, TT], BF16)
                    with kb.scope():
                        xt = [kb.sb(f"oxt{i}", [128, D], F32) for i in range(2)]
                        junk = kb.sb("ojunk", [128, D], BF16)
                        xn = kb.sb("oxn", [128, D], F32)
                        ss = [kb.sb(f"oss{i}", [128, 4], F32) for i in range(2)]
                        for t in range(NT):
                            w = 1 if t < 2 else 0
                            x_ = xt[t % 2]
                            kb.dma("sp", x_[:], self.xtile_src(src, t), reads=[src[2].regs[t]], writes=[x_])
                            self.norm_tile(x_, A1[:, w, :], modT[:, w, 0:8], hnT, t * 128, (PS[0], PS[1]), (junk, ss[t % 2], xn))
                    wv = inp["od_w_in"].t[0].rearrange("(kc p) n -> p kc n", p=128)
                    wb = [kb.sb(f"owb{i}", [128, 8, 512], BF16) for i in range(2)]
                    cw = kb.sb("ocw", [128, 24, 7], F32)
                    cwst = kb.sb("ocwst", [7, 3072], F32)
                    kb.dma("sp", cwst[:], inp["od_conv_w"][0], writes=[cwst])
                    for j_ in range(24):
                        kb.op("pe", lambda: nc.tensor.transpose(PS[7][:, j_ * 7:(j_ + 1) * 7], cwst[0:7, j_ * 128:(j_ + 1) * 128], self.ident[0:7, 0:7]),
                              reads=[cwst, self.ident], writes=[PS[7]], signal=(j_ == 23))
                    kb.op("dve", lambda: nc.vector.tensor_copy(out=cw[:].rearrange("p c k -> p (c k)"), in_=PS[7][:, 0:168]), reads=[PS[7]], writes=[cw])
                    cbias = kb.sb("ocb", [128, 24], F32)
                    self.colT(cbias[:], inp["od_conv_b"][0, :].rearrange("(c p) -> c p", p=128), 24, ps=PS[6], wbuf=cbias)
                    pre_l = [kb.sb(f"opre{i}", [128, TT + 12], BF16) for i in range(2)]
                    for pb_ in pre_l:
                        kb.op("pool", lambda: nc.gpsimd.memset(pb_[:], 0.0), writes=[pb_])
                    dg_l = [kb.sb(f"odiag{i}", [128, 7, 128], BF16) for i in range(2)]
                    pcol = lambda u: 3 + u if u < TC else 9 + u
                    postf_l = [kb.sb(f"opostf{i}", [128, TT], F32) for i in range(2)]
                    postb_l = [kb.sb(f"opostb{i}", [128, TT], BF16) for i in range(2)]
                    zst = [kb.sb(f"ozst{i}", [128, T], BF16) for i in range(2)]
                    tokst = [kb.sb(f"otok{i}", [128, NT, 128], BF16) for i in range(2)]
                    ipp = 0
                    itk = 0
                    def o1_load(blk_):
                        for hh in range(2):
                            kb.dma("pool", wb[blk_ % 2][:, :, hh * 256:(hh + 1) * 256],
                                   wv[:, :, blk_ * 512 + hh * 256:blk_ * 512 + (hh + 1) * 256], writes=[wb[blk_ % 2]])

                    o1_load(0)
                    for blk in range(10):
                        wb_ = wb[blk % 2]
                        c0 = blk * 512
                        if blk + 1 < 10:
                            o1_load(blk + 1)
                        for c in range(4):
                            fc = blk * 4 + c
                            is_z = fc < 16
                            blocks = LAT_BLOCKS if is_z else BLOCKS
                            zs_ = zst[fc % 2]
                            pre, postf, postb, dg = pre_l[fc % 2], postf_l[fc % 2], postb_l[fc % 2], dg_l[fc % 2]
                            for (s0, n) in blocks:
                                pp = PS[2 + ipp % 3]
                                ipp += 1
                                for kc in range(8):
                                    kb.op("pe", lambda: nc.tensor.matmul(pp[:, 0:n], lhsT=wb_[:, kc, c * 128:(c + 1) * 128],
                                                                         rhs=hnT[:, kc, s0:s0 + n], start=(kc == 0), stop=(kc == 7)),
                                          reads=[wb_, hnT], writes=[pp], signal=(kc == 7))
                                if is_z:
                                    kb.op("act", lambda: nc.scalar.activation(out=zs_[:, s0 - TC:s0 - TC + n], in_=pp[:, 0:n], func=AF.Silu),
                                          reads=[pp], writes=[zs_])
                                else:
                                    kb.op("act", lambda: nc.scalar.activation(out=pre[:, pcol(s0):pcol(s0) + n], in_=pp[:, 0:n], func=AF.Copy),
                                          reads=[pp], writes=[pre])
                            if is_z:
                                kb.dma("sp", zsT[fc], zs_[:], reads=[zs_], writes=[zsT])
                                continue
                            cc = fc - 16
                            for k in range(7):
                                kb.op("dve", lambda: nc.vector.tensor_scalar(out=dg[:, k, :], in0=self.ident[:], scalar1=cw[:, cc, k:k + 1],
                                                                             scalar2=None, op0=ALU.mult), reads=[self.ident, cw], writes=[dg])
                            for bi_, (s0, n) in enumerate(BLOCKS):
                                pcv = PS[bi_ % 2]
                                for k in range(7):
                                    kb.op("pe", lambda: nc.tensor.matmul(pcv[:, 0:n], lhsT=dg[:, k, :],
                                                                         rhs=pre[:, pcol(s0) + k - 3:pcol(s0) + k - 3 + n],
                                                                         start=(k == 0), stop=(k == 6)),
                                          reads=[dg, pre], writes=[pcv], signal=(k == 6))
                                kb.op("act", lambda: nc.scalar.activation(out=postf[:, s0:s0 + n], in_=pcv[:, 0:n], func=AF.Silu,
                                                                          bias=cbias[:, cc:cc + 1]), reads=[pcv, cbias], writes=[postf])
                            kb.op("pool", lambda: nc.gpsimd.tensor_copy(out=postb[:], in_=postf[:]), reads=[postf], writes=[postb])
                            if cc < 16:
                                kb.dma("sp", xsT[cc], postb[:], reads=[postb], writes=[xsT])
                            elif cc < 20:
                                kb.dma("sp", BT[cc - 16], postb[:], reads=[postb], writes=[BT])
                            else:
                                kb.dma("sp", CT[cc - 20], postb[:], reads=[postb], writes=[CT])
                            if cc < 20:
                                tk = tokst[itk % 2]
                                itk += 1
                                for t4 in range(0, NT, 4):
                                    nt4 = min(4, NT - t4)
                                    ptr = PS[5 + (t4 // 4) % 3]
                                    for q_ in range(nt4):
                                        t = t4 + q_
                                        kb.op("pe", lambda: nc.tensor.transpose(ptr[:, q_ * 128:(q_ + 1) * 128], postf[:, t * 128:(t + 1) * 128],
                                                                                self.ident[:]),
                                              reads=[postf, self.ident], writes=[ptr], signal=(q_ == nt4 - 1))
                                    kb.op("act", lambda: nc.scalar.activation(
                                        out=tk[:, t4:t4 + nt4, :], in_=ptr[:, 0:nt4 * 128].rearrange("p (t f) -> p t f", f=128), func=AF.Copy),
                                        reads=[ptr], writes=[tk])
                                if cc < 16:
                                    kb.dma("sp", xs_tok[:, :, cc * 128:(cc + 1) * 128].rearrange("t p f -> p t f"), tk[:], reads=[tk], writes=[xs_tok])
                                else:
                                    g_ = cc - 16
                                    kb.dma("sp", B_tok[:, :, g_ * 128:(g_ + 1) * 128].rearrange("t p f -> p t f"), tk[:], reads=[tk], writes=[B_tok])
                    wdt = kb.sb("owdt", [128, 8, 64], BF16)
                    kb.dma("pool", wdt[:], wv[:, :, 5120:5184], writes=[wdt])
                    dtb = kb.sb("odtb", [128, 64], F32)
                    Abc = kb.sb("oAbc", [128, 64], F32)
                    for di, (nb, na) in enumerate((("od_dt_bias_f", "od_a_log_f"), ("od_dt_bias_b", "od_a_log_b"))):
                        kb.dma("sp", dtb[:, di * 32:(di + 1) * 32], inp[nb][0:1, :].to_broadcast([128, 32]), writes=[dtb])
                        kb.dma("sp", Abc[:, di * 32:(di + 1) * 32], inp[na][0:1, :].to_broadcast([128, 32]), writes=[Abc])
                    kb.op("act", lambda: nc.scalar.activation(out=Abc[:], in_=Abc[:], func=AF.Exp), reads=[Abc], writes=[Abc])
                    kb.op("dve", lambda: nc.vector.tensor_scalar(out=Abc[:], in0=Abc[:], scalar1=-1.0, scalar2=None, op0=ALU.mult),
                          reads=[Abc], writes=[Abc])
                    for t in range(NT):
                        pd = PS[t % 2]
                        for kc in range(8):
                            kb.op("pe", lambda: nc.tensor.matmul(pd[:, 0:64], lhsT=hnT[:, kc, t * 128:(t + 1) * 128], rhs=wdt[:, kc, :],
                                                                 start=(kc == 0), stop=(kc == 7)),
                                  reads=[hnT, wdt], writes=[pd], signal=(kc == 7))
                        kb.op("dve", lambda: nc.vector.tensor_tensor(out=dt_sb[:, t, :], in0=pd[:, 0:64], in1=dtb[:], op=ALU.add),
                              reads=[pd, dtb], writes=[dt_sb])
                    kb.op("act", lambda: nc.scalar.activation(out=dt_sb[:], in_=dt_sb[:], func=AF.Exp), reads=[dt_sb], writes=[dt_sb])
                    kb.op("dve", lambda: nc.vector.tensor_scalar(out=dt_sb[:], in0=dt_sb[:], scalar1=1.0, scalar2=None, op0=ALU.add),
                          reads=[dt_sb], writes=[dt_sb])
                    kb.op("act", lambda: nc.scalar.activation(out=dt_sb[:], in_=dt_sb[:], func=AF.Ln), reads=[dt_sb], writes=[dt_sb])
                    kb.op("dve", lambda: nc.vector.tensor_tensor(out=a_sb[:], in0=dt_sb[:], in1=Abc[:].unsqueeze(1).to_broadcast([128, NT, 64]),
                                                                 op=ALU.mult), reads=[dt_sb, Abc], writes=[a_sb])
                if "od_dt" in self.debug:
                    dd = self.dbg_out("od_dt", [128, NT, 64])
                    kb.dma("sp", dd[:], dt_sb[:], reads=[dt_sb], writes=[dd])
                if "stop_o1" not in self.debug:
                    self.ssd(src, dt_sb, a_sb, zsT, xsT, BT, CT, xs_tok, B_tok, Sst, gbc, A2, modT, hn2T, gates, xmid)
            if not (self.debug & {"stop_o1", "stop_ssdA", "stop_ssd"}):
                self.moe(hn2T, gates, xmid, gbc, final_out)

    def ssd(self, src, dt_sb, a_sb, zsT, xsT, BT, CT, xs_tok, B_tok, Sst, gbc, A2, modT, hn2T, gates, xmid):
        kb, nc = self.kb, self.nc
        inp = self.inp
        PS = self.PS
        with kb.scope():
            triu = kb.sb("triu", [128, 128], F32)
            tril = kb.sb("tril", [128, 128], F32)
            ones_f = kb.sb("ones_f", [128, 128], F32)
            kb.dma("sp", triu[:], inp["cst_triu"][:], writes=[triu])
            kb.dma("sp", tril[:], inp["cst_tril"][:], writes=[tril])
            kb.dma("sp", ones_f[:], inp["cst_ones"][:], writes=[ones_f])
            TRI = (triu, tril)
            with kb.scope():
                S = [kb.sb(f"S{i}", [128, 2048], F32) for i in range(2)]
                Sbf = [kb.sb(f"Sbf{i}", [128, 2048], BF16) for i in range(2)]
                xtk = [kb.sb(f"axtk{i}", [128, 2048], BF16) for i in range(2)]
                btk = [kb.sb(f"abtk{i}", [128, 512], BF16) for i in range(2)]
                xw = [kb.sb(f"axw{i}", [128, 2048], BF16) for i in range(2)]
                sm = [kb.sb(f"asm{i}", [128, 4, 32], F32) for i in range(2)]
                it = 0
                orders = [list(range(NT)), [1, 0] + list(range(NT - 1, 1, -1))]
                for d in range(2):
                    kb.op("dve", lambda: nc.vector.memset(S[d][:], 0.0), writes=[S[d]])
                for step in range(NT):
                    for d in range(2):
                        t = orders[d][step]
                        i2 = it % 2
                        it += 1
                        if t >= 2:
                            kb.op("act", lambda: nc.scalar.activation(out=Sbf[i2][:], in_=S[d][:], func=AF.Copy), reads=[S[d]], writes=[Sbf[i2]])
                            kb.dma("sp", Sst[d, t], Sbf[i2][:], reads=[Sbf[i2]], writes=[Sst.regs[d * NT + t]])
                        kb.dma("sp", xtk[i2][:], xs_tok[t], reads=[xs_tok], writes=[xtk[i2]])
                        kb.dma("sp", btk[i2][:], B_tok[t], reads=[B_tok], writes=[btk[i2]])
                        a_t = a_sb[:, t, d * 32:(d + 1) * 32]
                        pc = PS[0]
                        kb.op("pe", lambda: nc.tensor.matmul(pc[:, 0:32], lhsT=TRI[d][:], rhs=a_t, start=True, stop=True),
                              reads=[TRI[d], a_sb], writes=[pc], signal=False)
                        kb.op("pe", lambda: nc.tensor.matmul(pc[:, 32:64], lhsT=ones_f[:], rhs=a_t, start=True, stop=True),
                              reads=[ones_f, a_sb], writes=[pc])
                        m_ = sm[i2]
                        kb.op("act", lambda: nc.scalar.activation(out=m_[:, 0, :], in_=pc[:, 32:64], func=AF.Copy), reads=[pc], writes=[m_])
                        kb.op("dve", lambda: nc.vector.tensor_tensor(out=m_[:, 1, :], in0=m_[:, 0, :], in1=pc[:, 0:32], op=ALU.subtract),
                              reads=[m_, pc], writes=[m_])
                        kb.op("act", lambda: nc.scalar.activation(out=m_[:, 2, :], in_=m_[:, 1, :], func=AF.Exp), reads=[m_], writes=[m_])
                        kb.op("dve", lambda: nc.vector.tensor_tensor(out=m_[:, 2, :], in0=m_[:, 2, :], in1=dt_sb[:, t, d * 32:(d + 1) * 32],
                                                                     op=ALU.mult), reads=[m_, dt_sb], writes=[m_])
                        kb.op("act", lambda: nc.scalar.activation(out=m_[:, 3, :], in_=m_[:, 0, :], func=AF.Exp), reads=[m_], writes=[m_])
                        kb.op("dve", lambda: nc.vector.tensor_tensor(
                            out=xw[i2][:].rearrange("p (h e) -> p h e", e=64), in0=xtk[i2][:].rearrange("p (h e) -> p h e", e=64),
                            in1=m_[:, 2, :].unsqueeze(2).to_broadcast([128, 32, 64]), op=ALU.mult),
                            reads=[xtk[i2], m_], writes=[xw[i2]])
                        for g in range(4):
                            kb.op("pe", lambda: nc.tensor.matmul(PS[1 + g][:, :], lhsT=btk[i2][:, g * 128:(g + 1) * 128],
                                                                 rhs=xw[i2][:, g * 512:(g + 1) * 512], start=True, stop=True),
                                  reads=[btk[i2], xw[i2]], writes=[PS[1 + g]])
                        kb.op("pool", lambda: nc.gpsimd.tensor_tensor(
                            out=S[d][:].rearrange("p (h e) -> p h e", e=64), in0=S[d][:].rearrange("p (h e) -> p h e", e=64),
                            in1=m_[:, 3, :].unsqueeze(2).to_broadcast([128, 32, 64]), op=ALU.mult),
                            reads=[S[d], m_], writes=[S[d]])
                        for g in range(4):
                            kb.op("dve", lambda: nc.vector.tensor_tensor(out=S[d][:, g * 512:(g + 1) * 512], in0=PS[1 + g][:, :],
                                                                         in1=S[d][:, g * 512:(g + 1) * 512], op=ALU.add),
                                  reads=[PS[1 + g], S[d]], writes=[S[d]])
            if "stop_ssdA" in self.debug:
                return
            with kb.scope():
                w_out = kb.sb("w_out", [128, 16, D], BF16)
                wov = inp["od_w_out"].t[0].rearrange("(kc p) n -> p kc n", p=128)
                for i in range(4):
                    kb.dma("pool", w_out[:, i * 4:(i + 1) * 4, :], wov[:, i * 4:(i + 1) * 4, :], writes=[w_out])
                dsk = kb.sb("dsk", [128, 16], F32)
                dsv = inp["od_d_skip"][0, :].rearrange("(c two) -> two c", two=2)
                for hf_ in range(2):
                    kb.dma("sp", dsk[hf_ * 64:(hf_ + 1) * 64, :], dsv[hf_:hf_ + 1, :].to_broadcast([64, 16]), writes=[dsk],
                           allow_slow_non_contiguous=True)
                gn = kb.sb("gn", [128, 16], F32)
                self.colT(gn[:], inp["od_gnorm"][0, :].rearrange("(c p) -> c p", p=128), 16, ps=PS[6], wbuf=gn)
                wr = kb.sb("wr", [128, 8, 32], F32)
                kb.op("dve", lambda: nc.vector.memset(wr[:], 0.0), writes=[wr])
                kb.dma("sp", wr[:, :, 0:8], inp["od_router"][0].rearrange("(kc p) e -> p kc e", p=128), writes=[wr], allow_slow_non_contiguous=True)
                xtk = kb.sb("bxtk", [128, 2048], BF16)
                Sd = [kb.sb(f"bS{i}", [128, 2048], BF16) for i in range(2)]
                BTt = kb.sb("bBT", [128, 4, 128], BF16)
                CTt = kb.sb("bCT", [128, 4, 128], BF16)
                xsTt = kb.sb("bxsT", [128, 16, 128], BF16)
                zsTt = kb.sb("bzsT", [128, 16, 128], BF16)
                cbm = kb.sb("cbm", [128, 2, 4, 128], BF16)
                acs = kb.sb("acs", [128, 2, 32], F32)
                Rb = kb.sb("Rb", [128, 16, 128], F32)
                bc = kb.sb("bc", [128, 2, 32, 128], F32)
                Eb = [kb.sb(f"Eb{i}", [128, 128], F32) for i in range(4)]
                xdt = [kb.sb(f"bxdt{i}", [128, 2048], BF16) for i in range(2)]
                E_ = [kb.sb(f"E{i}", [128, 128], F32) for i in range(4)]
                MT = [kb.sb(f"MT{i}", [128, 128], BF16) for i in range(4)]
                Cs = [kb.sb(f"Cs{i}", [128, 128], BF16) for i in range(4)]
                v1 = [kb.sb(f"v1{i}", [128, 128], F32) for i in range(2)]
                v2 = [kb.sb(f"v2{i}", [128, 128], F32) for i in range(2)]
                sq = [kb.sb(f"bsq{i}", [128, 128], BF16) for i in range(2)]
                vgT = kb.sb("vgT", [128, 16, 128], BF16)
                rs = kb.sb("brs", [128, 2], F32)
                xr = kb.sb("bxr", [128, D], F32)
                tm = kb.sb("btm", [128, D], F32)
                junk = kb.sb("bjunk", [128, D], BF16)
                xn = kb.sb("bxn", [128, D], F32)
                ss = kb.sb("bss", [128, 4], F32)
                hf = kb.sb("bhf", [128, 8, 128], F32)
                lg = kb.sb("blg", [128, 8], F32)
                g8 = kb.sb("bg8", [128, 4, 8], F32)
                gs = kb.sb("bgs", [128, 4], F32)
                ih = 0
                stage = 99
                for d_ in self.debug:
                    if d_.startswith("stopb_"):
                        stage = int(d_[6:])
                for t in range(2, NT if stage == 99 else 3):
                    lt = t - 2
                    kb.dma("sp", xtk[:], xs_tok[t], reads=[xs_tok], writes=[xtk])
                    for d in range(2):
                        kb.op("pool", lambda: nc.gpsimd.tensor_tensor(
                            out=xdt[d][:].rearrange("p (h e) -> p h e", e=64), in0=xtk[:].rearrange("p (h e) -> p h e", e=64),
                            in1=dt_sb[:, t, d * 32:(d + 1) * 32].unsqueeze(2).to_broadcast([128, 32, 64]), op=ALU.mult),
                            reads=[xtk, dt_sb], writes=[xdt[d]])
                    for d in range(2):
                        kb.dma("sp", Sd[d][:], Sst[d, t], reads=[Sst.regs[d * NT + t]], writes=[Sd[d]])
                    kb.dma("sp", BTt[:], BT[:, :, t * 128:(t + 1) * 128].rearrange("g p l -> p g l"), reads=[BT], writes=[BTt])
                    kb.dma("sp", CTt[:], CT[:, :, t * 128:(t + 1) * 128].rearrange("g p l -> p g l"), reads=[CT], writes=[CTt])
                    kb.dma("sp", xsTt[:], xsT[:, :, t * 128:(t + 1) * 128].rearrange("c p l -> p c l"), reads=[xsT], writes=[xsTt])
                    kb.dma("sp", zsTt[:], zsT[:, :, lt * 128:(lt + 1) * 128].rearrange("c p l -> p c l"), reads=[zsT], writes=[zsTt])
                    if stage < 2:
                        continue
                    pcb = PS[4]
                    for g in range(4):
                        kb.op("pe", lambda: nc.tensor.matmul(pcb[:, g * 128:(g + 1) * 128], lhsT=BTt[:, g, :], rhs=CTt[:, g, :], start=True, stop=True),
                              reads=[BTt, CTt], writes=[pcb], signal=(g == 3))
                    for d in range(2):
                        kb.op("dve", lambda: nc.vector.tensor_tensor(out=cbm[:, d, :, :], in0=pcb[:, :].rearrange("p (g l) -> p g l", l=128),
                                                                     in1=TRI[d][:].unsqueeze(1).to_broadcast([128, 4, 128]), op=ALU.mult),
                              reads=[pcb, TRI[d]], writes=[cbm])
                    pc = PS[7]
                    for d in range(2):
                        a_t = a_sb[:, t, d * 32:(d + 1) * 32]
                        kb.op("pe", lambda: nc.tensor.matmul(pc[:, d * 32:(d + 1) * 32], lhsT=TRI[d][:], rhs=a_t, start=True, stop=True),
                              reads=[TRI[d], a_sb], writes=[pc])
                    kb.op("act", lambda: nc.scalar.activation(out=acs[:].rearrange("p d h -> p (d h)"), in_=pc[:, 0:64], func=AF.Copy, scale=-1.0),
                          reads=[pc], writes=[acs])
                    ib = 0
                    for d in range(2):
                        for hh in range(2):
                            kb.op("pool", lambda: nc.gpsimd.tensor_tensor(
                                out=Rb[:], in0=a_sb[:, t, d * 32 + hh * 16:d * 32 + (hh + 1) * 16].unsqueeze(2).to_broadcast([128, 16, 128]),
                                in1=TRI[d][:].unsqueeze(1).to_broadcast([128, 16, 128]), op=ALU.mult),
                                reads=[a_sb, TRI[d], Rb], writes=[Rb])
                            for q_ in range(4):
                                pb = PS[5 + ib % 2]
                                ib += 1
                                kb.op("pe", lambda: nc.tensor.matmul(pb[:, :], lhsT=ones_f[:], rhs=Rb[:, q_ * 4:(q_ + 1) * 4, :].rearrange("p h l -> p (h l)"),
                                                                     start=True, stop=True), reads=[ones_f, Rb], writes=[pb])
                                kb.op("act", lambda: nc.scalar.activation(
                                    out=bc[:, d, hh * 16 + q_ * 4:hh * 16 + (q_ + 1) * 4, :], in_=pb[:, :].rearrange("p (h l) -> p h l", l=128), func=AF.Copy),
                                    reads=[pb], writes=[bc])
                    if stage < 3:
                        continue
                    for h in range(32):
                        g = h // 8
                        ybank = PS[h // 8]
                        yreg = ybank[(h % 2) * 64:(h % 2) * 64 + 64, ((h // 2) % 4) * 128:((h // 2) % 4) * 128 + 128]
                        for d in range(2):
                            i2 = ih % 4
                            i3 = ih % 4
                            ih += 1
                            kb.op("act", lambda: nc.scalar.activation(out=E_[i2][:], in_=bc[:, d, h, :], func=AF.Exp, bias=acs[:, d, h:h + 1]),
                                  reads=[bc, acs], writes=[E_[i2]])
                            kb.op("dve", lambda: nc.vector.scalar_tensor_tensor(out=MT[i3][:], in0=E_[i2][:], scalar=1.0,
                                                                                in1=cbm[:, d, g, :], op0=ALU.min, op1=ALU.mult),
                                  reads=[E_[i2], cbm], writes=[MT[i3]])
                            kb.op("act", lambda: nc.scalar.activation(out=Eb[i2][:], in_=bc[:, d, h, :], func=AF.Exp), reads=[bc], writes=[Eb[i2]])
                            kb.op("pool", lambda: nc.gpsimd.tensor_tensor(out=Cs[i3][:], in0=CTt[:, g, :], in1=Eb[i2][:], op=ALU.mult),
                                  reads=[CTt, Eb[i2]], writes=[Cs[i3]])
                            kb.op("pe", lambda: nc.tensor.matmul(yreg, lhsT=xdt[d][:, h * 64:(h + 1) * 64], rhs=MT[i3][:], start=(d == 0), stop=False),
                                  reads=[xdt[d], MT[i3]], writes=[ybank], signal=False)
                            kb.op("pe", lambda: nc.tensor.matmul(yreg, lhsT=Sd[d][:, h * 64:(h + 1) * 64], rhs=Cs[i3][:], start=False, stop=(d == 1)),
                                  reads=[Sd[d], Cs[i3]], writes=[ybank])
                    if stage < 4:
                        continue
                    pss = PS[7]
                    for fc in range(16):
                        i2 = fc % 2
                        yr = PS[fc // 4][:, (fc % 4) * 128:(fc % 4 + 1) * 128]
                        kb.op("dve", lambda: nc.vector.scalar_tensor_tensor(out=v1[i2][:], in0=xsTt[:, fc, :], scalar=dsk[:, fc:fc + 1], in1=yr,
                                                                            op0=ALU.mult, op1=ALU.add),
                              reads=[xsTt, dsk, PS[fc // 4]], writes=[v1[i2]])
                        kb.op("pool", lambda: nc.gpsimd.tensor_tensor(out=v2[i2][:], in0=v1[i2][:], in1=zsTt[:, fc, :], op=ALU.mult),
                              reads=[v1[i2], zsTt], writes=[v2[i2]])
                        kb.op("act", lambda: nc.scalar.activation(out=sq[i2][:], in_=v2[i2][:], func=AF.Square), reads=[v2[i2]], writes=[sq[i2]])
                        kb.op("act", lambda: nc.scalar.activation(out=vgT[:, fc, :], in_=v2[i2][:], func=AF.Identity, scale=gn[:, fc:fc + 1]),
                              reads=[v2[i2], gn], writes=[vgT])
                        kb.op("pe", lambda: nc.tensor.matmul(pss[:, 64:66], lhsT=sq[i2][:], rhs=self.ones_bf[:, 0:2], start=(fc == 0), stop=(fc == 15)),
                              reads=[sq[i2], self.ones_bf], writes=[pss])
                    kb.op("act", lambda: nc.scalar.activation(out=rs[:, 0:1], in_=pss[:, 64:65], func=AF.Sqrt, scale=1.0 / 2048, bias=self.eps_t[:, 0:1]),
                          reads=[pss, self.eps_t], writes=[rs])
                    kb.op("dve", lambda: nc.vector.reciprocal(out=rs[:, 1:2], in_=rs[:, 0:1]), reads=[rs], writes=[rs])
                    kb.dma("sp", xr[:], self.xtile_src(src, t), reads=[src[2].regs[t]], writes=[xr])
                    for hlf in range(2):
                        po = PS[5 + hlf]
                        for fc in range(16):
                            kb.op("pe", lambda: nc.tensor.matmul(po[:, :], lhsT=vgT[:, fc, :], rhs=w_out[:, fc, hlf * 512:(hlf + 1) * 512],
                                                                 start=(fc == 0), stop=(fc == 15)), reads=[vgT, w_out], writes=[po], signal=(fc == 15))
                        kb.op("act", lambda: nc.scalar.activation(out=tm[:, hlf * 512:(hlf + 1) * 512], in_=po[:, :], func=AF.Identity, scale=rs[:, 1:2]),
                              reads=[po, rs], writes=[tm])
                    kb.op("dve", lambda: nc.vector.tensor_tensor(out=tm[:], in0=tm[:], in1=gbc[:, 0, 0, :], op=ALU.mult), reads=[tm, gbc], writes=[tm])
                    kb.op("pool", lambda: nc.gpsimd.tensor_tensor(out=xr[:], in0=xr[:], in1=tm[:], op=ALU.add), reads=[xr, tm], writes=[xr])
                    kb.dma("sp", xmid[lt * 128:(lt + 1) * 128, :], xr[:], reads=[xr], writes=[xmid.regs[lt]])
                    if stage < 5:
                        continue
                    plg = PS[5]
                    self.norm_tile(xr, A2[:, 0, :], modT[:, 0, 24:32], hn2T, lt * 128, (PS[4], PS[7]), (junk, ss, xn), router=None if 'norouter' in self.debug else (wr, hf, plg))
                    kb.op("act", lambda: nc.scalar.activation(out=lg[:], in_=plg[:, 0:8], func=AF.Copy), reads=[plg], writes=[lg])
                    if "od_logits" in self.debug:
                        if lt == 0:
                            self._dlg = self.dbg_out("od_logits", [T, 8])
                        kb.dma("sp", self._dlg[lt * 128:(lt + 1) * 128, :], lg[:], reads=[lg], writes=[])
                    if stage < 6:
                        continue
                    kb.op("dve", lambda: nc.vector.max(out=g8[:, 0, :], in_=lg[:]), reads=[lg], writes=[g8])
                    kb.op("dve", lambda: nc.vector.tensor_scalar(out=g8[:, 1, :], in0=lg[:], scalar1=g8[:, 0, 1:2], scalar2=None, op0=ALU.is_ge),
                          reads=[lg, g8], writes=[g8])
                    kb.op("dve", lambda: nc.vector.tensor_scalar(out=gs[:, 0:1], in0=g8[:, 0, 0:1], scalar1=-1.0, scalar2=None, op0=ALU.mult),
                          reads=[g8], writes=[gs])
                    kb.op("act", lambda: nc.scalar.activation(out=g8[:, 2, :], in_=lg[:], func=AF.Exp, bias=gs[:, 0:1]), reads=[lg, gs], writes=[g8])
                    kb.op("dve", lambda: nc.vector.tensor_tensor(out=g8[:, 3, :], in0=g8[:, 2, :], in1=g8[:, 1, :], op=ALU.mult), reads=[g8], writes=[g8])
                    kb.op("dve", lambda: nc.vector.tensor_reduce(out=gs[:, 1:2], in_=g8[:, 3, :], axis=AX.X, op=ALU.add), reads=[g8], writes=[gs])
                    kb.op("dve", lambda: nc.vector.reciprocal(out=gs[:, 2:3], in_=gs[:, 1:2]), reads=[gs], writes=[gs])
                    kb.op("dve", lambda: nc.vector.tensor_scalar(out=gates[:, lt, :], in0=g8[:, 3, :], scalar1=gs[:, 2:3], scalar2=None, op0=ALU.mult),
                          reads=[g8, gs], writes=[gates.regs[lt]])

    def moe(self, hn2T, gates, xmid, gbc, final_out):
        kb, nc = self.kb, self.nc
        inp = self.inp
        PS = self.PS
        with kb.scope():
            yacc = kb.sb("myacc", [128, 16, D], F32, nreg=16)
            gT = kb.sb("gT", [8, T], F32)
            sel8 = kb.sb("sel8", [8, 8, 128], F32)
            kb.dma("sp", sel8[:].rearrange("k e m -> k (e m)"), inp["cst_sel8"][:], writes=[sel8])
            for t4 in range(0, 16, 4):
                pt = PS[(t4 // 4) % 2]
                for q_ in range(4):
                    kb.op("pe", lambda: nc.tensor.transpose(pt[0:8, q_ * 128:(q_ + 1) * 128], gates[:, t4 + q_, :], self.ident[:]),
                          reads=[gates.regs[t4 + q_], self.ident], writes=[pt], signal=(q_ == 3))
                kb.op("act", lambda: nc.scalar.activation(out=gT[:, t4 * 128:(t4 + 4) * 128], in_=pt[0:8, :], func=AF.Copy), reads=[pt], writes=[gT])
            with kb.scope():
                gbcast = kb.sb("gbcast", [128, T], F32)
                state = {"e": None}

                def gate_fn(e):
                    if state["e"] != e:
                        state["e"] = e
                        for b4 in range(4):
                            pg = PS[6 + b4 % 2]
                            kb.op("pe", lambda: nc.tensor.matmul(pg[:, :], lhsT=sel8[:, e, :], rhs=gT[:, b4 * 512:(b4 + 1) * 512], start=True, stop=True),
                                  reads=[sel8, gT], writes=[pg])
                            kb.op("act", lambda: nc.scalar.activation(out=gbcast[:, b4 * 512:(b4 + 1) * 512], in_=pg[:, :], func=AF.Copy),
                                  reads=[pg], writes=[gbcast])
                    return gbcast

                wblocks = []
                for e in range(NEXP):
                    w1v = inp["od_ex_w1"].t[0, e].rearrange("(kc p) n -> p kc n", p=128)
                    w3v = inp["od_ex_w3"].t[0, e].rearrange("(kc p) n -> p kc n", p=128)
                    w2v = inp["od_ex_w2"].t[0, e].rearrange("(c p) n -> p c n", p=128)
                    for fb in range(D_FFE // 512):
                        wblocks.append((w1v, w3v, w2v, fb * 512, 512, e))
                self.ffn(hn2T, [(512 * i, 512) for i in range(4)], wblocks, yacc, gate_fn)
            self.final_residual(xmid, yacc, gbc, 1, final_out, list(range(16)), final_norm=self.inp["final_norm"], lat_only=True)


_CACHE = {}


def _build(shapes):
    P = Prog(shapes)
    P.load_consts()
    x1 = P.kb.dram("scr_x1", [TT, D], F32, "Internal", nreg=NT)
    P.even_layer((P.inp["ctx"].t, P.inp["x"].t), x1)
    P.odd_layer((x1.t[0:TC], x1.t[TC:], x1), P.out)
    P.kb.barrier()
    return P


def kernel(**inputs):
    consts = host_consts()
    f32 = lambda a: np.ascontiguousarray(np.asarray(a, dtype=np.float32))
    shared = {}
    for n in WEIGHT_NAMES:
        a = f32(inputs[n])
        shared[n] = a if a.ndim > 1 else a[None, :]
    shared["c_ctx"] = f32(inputs["c_ctx"])[None, :]
    shared.update(consts)
    x = f32(inputs["x"])
    ctx = f32(inputs["ctx"])
    c = f32(inputs["c"])
    nb = x.shape[0]
    in_maps = []
    for b in range(nb):
        m = {"x": x[b], "ctx": ctx[b], "c": c[b:b + 1]}
        m.update(shared)
        in_maps.append(m)
    shapes = {k: list(v.shape) for k, v in in_maps[0].items()}
    P = _build(shapes)
    res = run_bass_kernel_spmd(P.nc, in_maps, core_ids=list(range(nb)))
    return np.stack([np.asarray(r["out"], dtype=np.float32) for r in res.results], axis=0)
```

```python
import numpy as np
from contextlib import ExitStack
import concourse.bass as bass
import concourse.mybir as mybir
from concourse.bass_utils import run_bass_kernel_spmd

F32 = mybir.dt.float32
BF16 = mybir.dt.bfloat16
ALU = mybir.AluOpType
AF = mybir.ActivationFunctionType
AX = mybir.AxisListType

D = 1024
T = 2048
TC = 256
TT = T + TC
NT = TT // 128
EPS = 1e-6
BLOCKS = [(0, 256)] + [(256 + 512 * i, 512) for i in range(4)]
LAT_BLOCKS = BLOCKS[1:]
D_FF = 2816
D_FFE = 3584
NEXP = 8


class Reg:
    __slots__ = ("w", "r")

    def __init__(self):
        self.w = []
        self.r = []


class Buf:
    def __init__(self, t, nreg=0):
        self.t = t
        self.reg = Reg()
        self.regs = [Reg() for _ in range(nreg)]

    def __getitem__(self, idx):
        return self.t[idx]


SEM_LIMIT = 12000


class KB:
    ENG = ("pe", "act", "dve", "pool", "sp")

    def __init__(self, nc, n_dma_sems=24):
        self.nc = nc
        self.es = ExitStack()
        self.e = {"pe": nc.tensor, "act": nc.scalar, "dve": nc.vector, "pool": nc.gpsimd, "sp": nc.sync}
        self.sems = {}
        self.owner = {}
        self.cur = {}
        self.nsem = 0
        for en in self.ENG:
            self._new_eng_sem(en)
        self.waited = {en: {} for en in self.ENG}
        self.dq = {}
        for q in ("sp", "pool"):
            lst = []
            for i in range(n_dma_sems):
                k = f"d_{q}{i}"
                self.sems[k] = self.es.enter_context(nc.semaphore(k))
                self.owner[k] = None
                lst.append([k, 0])
            self.dq[q] = [lst, 0]
        self.scopes = []
        self.ninst = 0

    def _new_eng_sem(self, en):
        k = f"s_{en}{self.nsem}"
        self.nsem += 1
        self.sems[k] = self.es.enter_context(self.nc.semaphore(k))
        self.owner[k] = en
        self.cur[en] = [k, 0]

    def scope(self):
        return _Scope(self)

    def _ctx(self):
        return self.scopes[-1] if self.scopes else self.es

    def sb(self, name, shape, dt, nreg=0):
        self.ninst += 0
        self.nalloc = getattr(self, "nalloc", 0) + 1
        name = f"{name}_{self.nalloc}"
        return Buf(self._ctx().enter_context(self.nc.sbuf_tensor(name, list(shape), dt)), nreg)

    def ps(self, name, shape, dt, nreg=0):
        return Buf(self._ctx().enter_context(self.nc.psum_tensor(name, list(shape), dt)), nreg)

    def dram(self, name, shape, dt, kind="Internal", nreg=0):
        t = self.nc.dram_tensor(name, list(shape), dt, kind=kind)
        return Buf(t.ap(), nreg)

    def _wait(self, en, evs):
        need = {}
        for (k, v) in evs:
            if self.owner[k] == en:
                ck, cv = self.cur[en]
                if en == "pe" or k != ck or v > cv:
                    continue
            if self.waited[en].get(k, 0) >= v:
                continue
            if need.get(k, 0) < v:
                need[k] = v
        for k, v in need.items():
            self.e[en].wait_ge(self.sems[k], v)
            self.waited[en][k] = v

    @staticmethod
    def _regs(lst):
        out = []
        for b in lst:
            if isinstance(b, Buf):
                out.append(b.reg)
            elif isinstance(b, (list, tuple)):
                out += KB._regs(b)
            else:
                out.append(b)
        return out

    def op(self, en, fn, reads=(), writes=(), signal=True):
        R = self._regs(reads)
        W = self._regs(writes)
        evs = []
        for r in R:
            evs += r.w
        for w in W:
            evs += w.w
            evs += w.r
        self._wait(en, evs)
        cur = self.cur[en]
        if signal and cur[1] >= SEM_LIMIT:
            self._new_eng_sem(en)
            cur = self.cur[en]
        ev = (cur[0], cur[1] + 1)
        inst = fn()
        self.ninst += 1
        if signal:
            inst.then_inc(self.sems[cur[0]], 1)
            cur[1] += 1
        for r in R:
            r.r = [x for x in r.r if x[0] != ev[0]] + [ev]
        for w in W:
            w.w = [ev]
            w.r = []
        return inst

    def dma(self, q, out, in_, reads=(), writes=(), **kw):
        R = self._regs(reads)
        W = self._regs(writes)
        lst, idx = self.dq[q]
        slot = lst[idx % len(lst)]
        self.dq[q][1] += 1
        evs = [(slot[0], slot[1])] if slot[1] > 0 else []
        for r in R:
            evs += r.w
        for w in W:
            evs += w.w
            evs += w.r
        self._wait(q, evs)
        slot[1] += 16
        ev = (slot[0], slot[1])
        self.e[q].dma_start(out=out, in_=in_, **kw).then_inc(self.sems[slot[0]], 16)
        self.ninst += 1
        for r in R:
            r.r = r.r + [ev]
        for w in W:
            w.w = [ev]
            w.r = []
        return ev

    def barrier(self):
        evs = []
        for en in self.ENG:
            k, v = self.cur[en]
            if v > 0:
                evs.append((k, v))
        for q in self.dq:
            for k, v in self.dq[q][0]:
                if v > 0:
                    evs.append((k, v))
        for en in self.ENG:
            self._wait(en, evs)


class _Scope:
    def __init__(self, kb):
        self.kb = kb

    def __enter__(self):
        es = ExitStack()
        self.kb.scopes.append(es)
        return es

    def __exit__(self, *a):
        self.kb.barrier()
        es = self.kb.scopes.pop()
        es.close()
        return False


def host_consts():
    c = {}
    c["cst_ident"] = np.eye(128, dtype=np.float32)
    blk = np.zeros((128, 128), np.float32)
    blk[:64, :64] = 1.0
    blk[64:, 64:] = 1.0
    c["cst_blk64"] = blk
    c["cst_ones"] = np.ones((128, 128), np.float32)
    rot = np.zeros((128, 128), np.float32)
    for i in range(64):
        rot[2 * i + 1, 2 * i] = -1.0
        rot[2 * i, 2 * i + 1] = 1.0
    c["cst_rot"] = rot
    rows = T // 64
    t_row = np.repeat(np.arange(rows, dtype=np.float32), 64)
    t_col = np.tile(np.arange(64, dtype=np.float32), rows)
    inv_freq = (10000.0 ** (-np.arange(0, 32, 2, dtype=np.float32) / 32)).astype(np.float32)
    ang = np.concatenate([t_row[:, None] * inv_freq, t_col[:, None] * inv_freq], axis=-1)
    cos = np.cos(ang).astype(np.float32)
    sin = np.sin(ang).astype(np.float32)
    pidx = (np.arange(128) % 64) // 2
    c["cst_cos"] = np.ascontiguousarray(cos[:, pidx].T)
    c["cst_sin"] = np.ascontiguousarray(sin[:, pidx].T)
    s_ = np.arange(128)[:, None]
    l_ = np.arange(128)[None, :]
    c["cst_triu"] = (l_ >= s_).astype(np.float32)
    c["cst_tril"] = (l_ <= s_).astype(np.float32)
    sel8 = np.zeros((8, 8, 128), np.float32)
    for e in range(8):
        sel8[e, e, :] = 1.0
    c["cst_sel8"] = sel8.reshape(8, 1024)
    return c


WEIGHT_NAMES = [
    "ev_ada_w", "ev_ada_b", "ev_norm1", "ev_norm2", "ev_w_in", "ev_q_gain", "ev_k_gain", "ev_dw_w", "ev_dw_b",
    "ev_ln_g", "ev_ln_b", "ev_w_o", "ev_ff_w1", "ev_ff_w3", "ev_ff_w2",
    "od_ada_w", "od_ada_b", "od_norm1", "od_norm2", "od_w_in", "od_conv_w", "od_conv_b", "od_a_log_f", "od_a_log_b",
    "od_dt_bias_f", "od_dt_bias_b", "od_d_skip", "od_gnorm", "od_w_out", "od_router", "od_ex_w1", "od_ex_w3",
    "od_ex_w2", "final_norm",
]


class Prog:
    def __init__(self, shapes, debug=()):
        self.nc = bass.Bass("TRN2", target_bir_lowering=False)
        self.kb = KB(self.nc)
        self.debug = set(debug)
        kb = self.kb
        self.inp = {}
        for name, shp in shapes.items():
            self.inp[name] = kb.dram(name, shp, F32, "ExternalInput")
        self.out = kb.dram("out", [T, D], F32, "ExternalOutput", nreg=T // 128)
        self.dbg = {}

    def dbg_out(self, name, shape, dt=F32):
        kind = "ExternalOutput" if name in self.debug else "Internal"
        b = self.kb.dram("dbg_" + name if kind == "ExternalOutput" else "scr_" + name, shape, dt, kind)
        return b

    def load_consts(self):
        kb, nc = self.kb, self.nc
        self.ident = kb.sb("ident", [128, 128], F32)
        kb.dma("sp", self.ident[:], self.inp["cst_ident"][:], writes=[self.ident])
        self.blk64 = kb.sb("blk64", [128, 128], BF16)
        kb.dma("pool", self.blk64[:], self.inp["cst_blk64"][:], writes=[self.blk64])
        self.ones_bf = kb.sb("ones_bf", [128, 128], BF16)
        kb.dma("pool", self.ones_bf[:], self.inp["cst_ones"][:], writes=[self.ones_bf])
        self.rot = kb.sb("rot", [128, 128], BF16)
        kb.dma("pool", self.rot[:], self.inp["cst_rot"][:], writes=[self.rot])
        self.PS = [kb.ps(f"psb{i}", [128, 512], F32) for i in range(8)]
        self.colT_st = kb.sb("colT_st", [48, 128], F32)
        self.eps_t = kb.sb("eps_t", [128, 1], F32)
        kb.op("dve", lambda: nc.vector.memset(self.eps_t[:], EPS), writes=[self.eps_t])

    def col_load(self, dst, src_row_ap, n):
        self.kb.dma("sp", dst, src_row_ap.rearrange("(c p) -> p c", p=128), writes=[], allow_slow_non_contiguous=True)

    def colT(self, dst, src2d, n, ps=None, wbuf=None):
        kb, nc = self.kb, self.nc
        st_ = self.colT_st
        kb.dma("sp", st_[0:n, :], src2d, writes=[st_])
        ps = self.PS[7] if ps is None else ps
        kb.op("pe", lambda: nc.tensor.transpose(ps[:, 0:n], st_[0:n, :], self.ident[0:n, 0:n]), reads=[st_, self.ident], writes=[ps])
        kb.op("dve", lambda: nc.vector.tensor_copy(out=dst, in_=ps[:, 0:n]), reads=[ps], writes=[wbuf])


    def xtile_src(self, src, t):
        ctx_ap, lat_ap = src[0], src[1]
        if t < 2:
            return ctx_ap[t * 128:(t + 1) * 128, :]
        return lat_ap[(t - 2) * 128:(t - 1) * 128, :]

    def norm_tile(self, xt, A, S, hnT, col0, pst, tmp, router=None):
        kb, nc = self.kb, self.nc
        junk, ss, xn = tmp
        kb.op("act", lambda: nc.scalar.activation(out=junk[:], in_=xt[:], func=AF.Square, accum_out=ss[:, 0:1]),
              reads=[xt], writes=[junk, ss])
        kb.op("act", lambda: nc.scalar.activation(out=ss[:, 1:2], in_=ss[:, 0:1], func=AF.Sqrt, scale=1.0 / D,
                                                  bias=self.eps_t[:, 0:1]), reads=[ss, self.eps_t], writes=[ss])
        kb.op("dve", lambda: nc.vector.reciprocal(out=ss[:, 2:3], in_=ss[:, 1:2]), reads=[ss], writes=[ss])
        kb.op("dve", lambda: nc.vector.tensor_scalar(out=xn[:], in0=xt[:], scalar1=ss[:, 2:3], scalar2=None,
                                                     op0=ALU.mult), reads=[xt, ss], writes=[xn])
        for half in range(2):
            ps = pst[half]
            for jj in range(4):
                j = half * 4 + jj
                kb.op("pe", lambda: nc.tensor.transpose(ps[:, jj * 128:(jj + 1) * 128], xn[:, j * 128:(j + 1) * 128],
                                                        self.ident[:]),
                      reads=[xn, self.ident], writes=[ps], signal=(jj == 3))
            for jj in range(4):
                j = half * 4 + jj
                kb.op("act", lambda: nc.scalar.activation(out=hnT[:, j, col0:col0 + 128],
                                                          in_=ps[:, jj * 128:(jj + 1) * 128], func=AF.Identity,
                                                          scale=A[:, j:j + 1], bias=S[:, j:j + 1]),
                      reads=[ps], writes=[hnT])
                if router is not None:
                    wr, hf, plg = router
                    kb.op("act", lambda: nc.scalar.activation(out=hf[:, j, :], in_=ps[:, jj * 128:(jj + 1) * 128], func=AF.Identity,
                                                              scale=A[:, j:j + 1], bias=S[:, j:j + 1]), reads=[ps], writes=[hf])
            if router is not None and half == 1:
                wr, hf, plg = router
                for j in range(8):
                    kb.op("pe", lambda: nc.tensor.matmul(plg[:, 0:32], lhsT=hf[:, j, :], rhs=wr[:, j, :], start=(j == 0),
                                                         stop=(j == 7)), reads=[hf, wr], writes=[plg], signal=(j == 7))

    def adaln(self, pre, norm1, norm2):
        kb, nc = self.kb, self.nc
        ada_w = self.inp[pre + "_ada_w"]
        ada_b = self.inp[pre + "_ada_b"]
        res = {}
        modT = kb.sb(pre + "modT", [128, 2, 48], F32)
        gbc = kb.sb(pre + "gbc", [128, 2, 2, 1024], F32)
        A1 = kb.sb(pre + "A1", [128, 2, 8], F32)
        A2 = kb.sb(pre + "A2", [128, 2, 8], F32)
        with kb.scope():
            cT = kb.sb("cT", [128, 2, 8], F32)
            self.colT(cT[:, 0, :], self.inp["c"][0, :].rearrange("(c p) -> c p", p=128), 8, ps=self.PS[5], wbuf=cT)
            self.colT(cT[:, 1, :], self.inp["c_ctx"][0, :].rearrange("(c p) -> c p", p=128), 8, ps=self.PS[6], wbuf=cT)
            sc = kb.sb("sc", [128, 2, 8], BF16)
            kb.op("act", lambda: nc.scalar.activation(out=sc[:], in_=cT[:], func=AF.Silu), reads=[cT], writes=[sc])
            scbc = kb.sb("scbc", [128, 2, 8, 128], BF16)
            for w in range(2):
                for kc in range(8):
                    kb.op("dve", lambda: nc.vector.tensor_copy(out=scbc[:, w, kc, :],
                                                               in_=sc[:, w, kc:kc + 1].to_broadcast([128, 128])),
                          reads=[sc], writes=[scbc])
            scr = kb.sb("scr", [128, 8, 2], BF16)
            for w in range(2):
                kb.op("dve", lambda: nc.vector.tensor_copy(out=scr[:, :, w], in_=sc[:, w, :]), reads=[sc], writes=[scr])
            bT = kb.sb("bT", [128, 48], F32)
            self.colT(bT[:], ada_b[0, :].rearrange("(c p) -> c p", p=128), 48, ps=self.PS[7], wbuf=bT)
            bbc = kb.sb("bbc", [128, 2, 1024], F32)
            for gi, c0 in enumerate((2048, 5120)):
                kb.dma("sp", bbc[:, gi, :], ada_b[0:1, c0:c0 + 1024].to_broadcast([128, 1024]), writes=[bbc])
            nT = kb.sb("nT", [128, 2, 8], F32)
            self.colT(nT[:, 0, :], norm1[0, :].rearrange("(c p) -> c p", p=128), 8, ps=self.PS[5], wbuf=nT)
            self.colT(nT[:, 1, :], norm2[0, :].rearrange("(c p) -> c p", p=128), 8, ps=self.PS[6], wbuf=nT)
            wv = ada_w.t[0].rearrange("(kc p) n -> p kc n", p=128)
            wbuf = [kb.sb(f"adaw{i}", [128, 8, 1024], BF16) for i in range(2)]
            pm = self.PS[0]
            for piece in range(6):
                wb = wbuf[piece % 2]
                for h in range(2):
                    kb.dma("pool", wb[:, :, h * 512:(h + 1) * 512], wv[:, :, piece * 1024 + h * 512:piece * 1024 + (h + 1) * 512],
                           writes=[wb])
                for fc in range(8):
                    j = piece * 8 + fc
                    for kc in range(8):
                        kb.op("pe", lambda: nc.tensor.matmul(pm[:, j * 2:j * 2 + 2], lhsT=wb[:, kc, fc * 128:(fc + 1) * 128],
                                                             rhs=scr[:, kc, :], start=(kc == 0), stop=(kc == 7)),
                              reads=[wb, scr], writes=[pm], signal=(kc == 7))
                if piece in (2, 5):
                    gi = 0 if piece == 2 else 1
                    for w in range(2):
                        for h in range(2):
                            pg = self.PS[1 + (w * 2 + h) % 4]
                            for kc in range(8):
                                kb.op("pe", lambda: nc.tensor.matmul(pg[:, :], lhsT=scbc[:, w, kc, :],
                                                                     rhs=wb[:, kc, h * 512:(h + 1) * 512],
                                                                     start=(kc == 0), stop=(kc == 7)),
                                      reads=[wb, scbc], writes=[pg], signal=(kc == 7))
                            kb.op("dve", lambda: nc.vector.tensor_tensor(out=gbc[:, w, gi, h * 512:(h + 1) * 512], in0=pg[:, :],
                                                                         in1=bbc[:, gi, h * 512:(h + 1) * 512], op=ALU.add),
                                  reads=[pg, bbc], writes=[gbc])
            for w in range(2):
                kb.op("dve", lambda: nc.vector.tensor_tensor(
                    out=modT[:, w, :], in0=pm[:, 0:96].rearrange("p (j w) -> p j w", w=2)[:, :, w], in1=bT[:], op=ALU.add),
                    reads=[pm, bT], writes=[modT])
                kb.op("dve", lambda: nc.vector.scalar_tensor_tensor(out=A1[:, w, :], in0=modT[:, w, 8:16], scalar=1.0,
                                                                    in1=nT[:, 0, :], op0=ALU.add, op1=ALU.mult),
                      reads=[modT, nT], writes=[A1])
                kb.op("dve", lambda: nc.vector.scalar_tensor_tensor(out=A2[:, w, :], in0=modT[:, w, 32:40], scalar=1.0,
                                                                    in1=nT[:, 1, :], op0=ALU.add, op1=ALU.mult),
                      reads=[modT, nT], writes=[A2])
        res["A1"] = A1
        res["A2"] = A2
        res["modT"] = modT
        res["gbc"] = gbc
        return res

    def even_layer(self, src, res_out):
        kb, nc = self.kb, self.nc
        inp = self.inp
        PS = self.PS
        with kb.scope():
            ada = self.adaln("ev", inp["ev_norm1"], inp["ev_norm2"])
            A1, A2, modT, gbc = ada["A1"], ada["A2"], ada["modT"], ada["gbc"]
            res1 = self.dbg_out("res1", [TT, D])
            res1.regs = [Reg() for _ in range(NT)]
            hn2T = kb.sb("hn2T", [128, 8, TT], BF16, nreg=NT)
            with kb.scope():
                qT = kb.sb("qT", [128, 4, TT], BF16, nreg=5)
                kT = kb.sb("kT", [128, 2, TT], BF16, nreg=5)
                kf = [kb.sb(f"kf{i}", [128, 512], F32) for i in range(2)]
                vaug = kb.sb("vaug", [128, NT, 2, 128], BF16, nreg=NT)
                glu = kb.sb("glu", [128, 4, TT], BF16, nreg=5)
                kb.op("pool", lambda: nc.gpsimd.memset(vaug[:], 1.0), writes=[vaug] + vaug.regs)
                with kb.scope():
                    w_in = kb.sb("w_in", [128, 8, 1792], BF16)
                    wv = inp["ev_w_in"].t[0].rearrange("(kc p) n -> p kc n", p=128)
                    for i in range(4):
                        kb.dma("pool", w_in[:, :, i * 448:(i + 1) * 448], wv[:, :, i * 448:(i + 1) * 448], writes=[w_in])
                    cos = kb.sb("cos", [128, T], F32)
                    sin = kb.sb("sin", [128, T], F32)
                    kb.dma("sp", cos[:], inp["cst_cos"][:], writes=[cos])
                    kb.dma("sp", sin[:], inp["cst_sin"][:], writes=[sin])
                    gain = kb.sb("gain", [128, 2], F32)
                    for hh in range(2):
                        kb.dma("sp", gain[hh * 64:(hh + 1) * 64, 0:1], inp["ev_q_gain"][0, :].rearrange("(p o) -> p o", o=1),
                               writes=[gain], allow_slow_non_contiguous=True)
                        kb.dma("sp", gain[hh * 64:(hh + 1) * 64, 1:2], inp["ev_k_gain"][0, :].rearrange("(p o) -> p o", o=1),
                               writes=[gain], allow_slow_non_contiguous=True)
                    xt = [kb.sb(f"xt{i}", [128, D], F32) for i in range(2)]
                    junk = kb.sb("junk", [128, D], BF16)
                    xn = [kb.sb(f"xn{i}", [128, D], F32) for i in range(1)] * 2
                    ss = [kb.sb(f"ss{i}", [128, 4], F32) for i in range(2)]
                    hnT = [kb.sb(f"hnT{i}", [128, 8, 512], BF16) for i in range(2)]
                    sq = [kb.sb(f"sq{i}", [128, 512], BF16) for i in range(2)]
                    rr = [kb.sb(f"rr{i}", [128, 512], F32) for i in range(2)]
                    qn = [kb.sb(f"qn{i}", [128, 512], F32) for i in range(2)]
                    qb = [kb.sb(f"qb{i}", [128, 512], BF16) for i in range(2)]
                    t1 = [kb.sb(f"t1{i}", [128, 512], F32) for i in range(1)] * 2
                    sg = [kb.sb(f"sg{i}", [128, 512], F32) for i in range(1)] * 2
                    tcount = 0
                    it = 0
                    for bi, (s0, n) in enumerate(BLOCKS):
                        hb = hnT[bi % 2]
                        w = 1 if bi == 0 else 0
                        for tl in range(n // 128):
                            t = s0 // 128 + tl
                            x_ = xt[tcount % 2]
                            kb.dma("sp", x_[:], self.xtile_src(src, t), writes=[x_])
                            self.norm_tile(x_, A1[:, w, :], modT[:, w, 0:8], hb, tl * 128, (PS[0], PS[1]),
                                           (junk, ss[tcount % 2], xn[tcount % 2]))
                            pv = PS[2]
                            for kc in range(8):
                                kb.op("pe", lambda: nc.tensor.matmul(pv[:, 0:128], lhsT=hb[:, kc, tl * 128:(tl + 1) * 128],
                                                                     rhs=w_in[:, kc, 640:768], start=(kc == 0), stop=(kc == 7)),
                                      reads=[hb, w_in], writes=[pv], signal=(kc == 7))
                            kb.op("dve", lambda: nc.vector.tensor_copy(
                                out=vaug[:, t, :, 0:64], in_=pv[:, 0:128].rearrange("p (g d) -> p g d", g=2)),
                                reads=[pv], writes=[vaug.regs[t]])
                            tcount += 1
                        for j in range(5):
                            pq = PS[3 + it % 2]
                            pss = PS[5]
                            ppq = PS[6]
                            i2 = it % 2
                            it += 1
                            for kc in range(8):
                                kb.op("pe", lambda: nc.tensor.matmul(pq[:, 0:n], lhsT=w_in[:, kc, j * 128:(j + 1) * 128],
                                                                     rhs=hb[:, kc, 0:n], start=(kc == 0), stop=(kc == 7)),
                                      reads=[hb, w_in], writes=[pq], signal=(kc == 7))
                            kb.op("act", lambda: nc.scalar.activation(out=sq[i2][:, 0:n], in_=pq[:, 0:n], func=AF.Square),
                                  reads=[pq], writes=[sq[i2]])
                            kb.op("pe", lambda: nc.tensor.matmul(pss[:, 0:n], lhsT=self.blk64[:], rhs=sq[i2][:, 0:n],
                                                                 start=True, stop=True),
                                  reads=[sq[i2], self.blk64], writes=[pss])
                            kb.op("act", lambda: nc.scalar.activation(out=rr[i2][:, 0:n], in_=pss[:, 0:n], func=AF.Sqrt,
                                                                      scale=1.0 / 64, bias=self.eps_t[:, 0:1]),
                                  reads=[pss, self.eps_t], writes=[rr[i2]])
                            kb.op("dve", lambda: nc.vector.reciprocal(out=rr[i2][:, 0:n], in_=rr[i2][:, 0:n]),
                                  reads=[rr[i2]], writes=[rr[i2]])
                            gcol = gain[:, 0:1] if j < 4 else gain[:, 1:2]
                            if j < 4:
                                dst = qT[:, j, s0:s0 + n]
                                dreg = qT.regs[bi]
                            else:
                                dst = kf[bi % 2][:, 0:n]
                                dreg = kf[bi % 2]
                            if bi == 0:
                                kb.op("dve", lambda: nc.vector.scalar_tensor_tensor(out=dst, in0=pq[:, 0:n], scalar=gcol,
                                                                                    in1=rr[i2][:, 0:n], op0=ALU.mult,
                                                                                    op1=ALU.mult),
                                      reads=[pq, gain, rr[i2]], writes=[dreg])
                            else:
                                l0 = s0 - TC
                                kb.op("dve", lambda: nc.vector.scalar_tensor_tensor(out=qn[i2][:, 0:n], in0=pq[:, 0:n],
                                                                                    scalar=gcol, in1=rr[i2][:, 0:n],
                                                                                    op0=ALU.mult, op1=ALU.mult),
                                      reads=[pq, gain, rr[i2]], writes=[qn[i2]])
                                kb.op("act", lambda: nc.scalar.activation(out=qb[i2][:, 0:n], in_=qn[i2][:, 0:n], func=AF.Copy),
                                      reads=[qn[i2]], writes=[qb[i2]])
                                kb.op("pe", lambda: nc.tensor.matmul(ppq[:, 0:n], lhsT=self.rot[:], rhs=qb[i2][:, 0:n],
                                                                     start=True, stop=True),
                                      reads=[qb[i2], self.rot], writes=[ppq])
                                kb.op("pool", lambda: nc.gpsimd.tensor_tensor(out=t1[i2][:, 0:n], in0=qn[i2][:, 0:n],
                                                                              in1=cos[:, l0:l0 + n], op=ALU.mult),
                                      reads=[qn[i2], cos], writes=[t1[i2]])
                                kb.op("dve", lambda: nc.vector.tensor_tensor(out=qn[i2][:, 0:n], in0=ppq[:, 0:n],
                                                                             in1=sin[:, l0:l0 + n], op=ALU.mult),
                                      reads=[ppq, sin, qn[i2]], writes=[qn[i2]])
                                kb.op("dve", lambda: nc.vector.tensor_tensor(out=dst, in0=qn[i2][:, 0:n], in1=t1[i2][:, 0:n],
                                                                             op=ALU.add),
                                      reads=[qn[i2], t1[i2]], writes=[dreg])
                        kfb = kf[bi % 2]
                        for g_ in range(2):
                            for hf in range(2):
                                kb.op("act", lambda: nc.scalar.activation(out=kT[hf * 64:(hf + 1) * 64, g_, s0:s0 + n],
                                                                          in_=kfb[g_ * 64:(g_ + 1) * 64, 0:n], func=AF.Copy),
                                      reads=[kfb], writes=[kT.regs[bi]])
                        for j in range(4):
                            pa = PS[3 + it % 2]
                            pg = PS[6 + it % 2]
                            i2 = it % 2
                            it += 1
                            for kc in range(8):
                                kb.op("pe", lambda: nc.tensor.matmul(pa[:, 0:n], lhsT=w_in[:, kc, 768 + j * 128:768 + (j + 1) * 128],
                                                                     rhs=hb[:, kc, 0:n], start=(kc == 0), stop=(kc == 7)),
                                      reads=[hb, w_in], writes=[pa], signal=(kc == 7))
                            for kc in range(8):
                                kb.op("pe", lambda: nc.tensor.matmul(pg[:, 0:n], lhsT=w_in[:, kc, 1280 + j * 128:1280 + (j + 1) * 128],
                                                                     rhs=hb[:, kc, 0:n], start=(kc == 0), stop=(kc == 7)),
                                      reads=[hb, w_in], writes=[pg], signal=(kc == 7))
                            kb.op("act", lambda: nc.scalar.activation(out=sg[i2][:, 0:n], in_=pg[:, 0:n], func=AF.Sigmoid),
                                  reads=[pg], writes=[sg[i2]])
                            kb.op("dve", lambda: nc.vector.tensor_tensor(out=glu[:, j, s0:s0 + n], in0=pa[:, 0:n],
                                                                         in1=sg[i2][:, 0:n], op=ALU.mult),
                                  reads=[pa, sg[i2]], writes=[glu.regs[bi]])
                if "ev_q" in self.debug:
                    dq = self.dbg_out("ev_q", [128, 4, TT], BF16)
                    kb.dma("sp", dq[:], qT[:], reads=qT.regs, writes=[dq])
                    dk = self.dbg_out("ev_k", [128, 2, TT], BF16)
                    kb.dma("sp", dk[:], kT[:], reads=kT.regs, writes=[dk])
                    dg = self.dbg_out("ev_glu", [128, 4, TT], BF16)
                    kb.dma("sp", dg[:], glu[:], reads=glu.regs, writes=[dg])
                    dv = self.dbg_out("ev_v", [128, NT, 2, 128], BF16)
                    kb.dma("sp", dv[:], vaug[:], reads=vaug.regs, writes=[dv])
                mix = kb.sb("mix", [128, 8, TT], BF16, nreg=5)
                with kb.scope():
                    cw = kb.sb("cw", [128, 4, 31], F32)
                    for j_ in range(4):
                        self.colT(cw[:, j_, :], inp["ev_dw_w"][0][:, j_ * 128:(j_ + 1) * 128], 31, ps=self.PS[4 + j_ % 2], wbuf=cw)
                    cb = kb.sb("cb", [128, 4, 3], F32)
                    for i, nm in enumerate(("ev_dw_b", "ev_ln_g", "ev_ln_b")):
                        self.colT(cb[:, :, i], inp[nm][0, :].rearrange("(c p) -> c p", p=128), 4, ps=self.PS[4 + i % 2], wbuf=cb)
                    cv = kb.sb("cv", [128, 4, TT], F32, nreg=4)
                    conv_ops = []

                    def mk_first(j, s0, n):
                        return lambda: kb.op("dve", lambda: nc.vector.tensor_scalar(
                            out=cv[:, j, s0:s0 + n], in0=glu[:, j, s0:s0 + n], scalar1=cw[:, j, 15:16], scalar2=cb[:, j, 0:1],
                            op0=ALU.mult, op1=ALU.add), reads=glu.regs + [cw, cb], writes=[cv.regs[j]])

                    def mk_tap(j, s0, lo, hi, o, k):
                        return lambda: kb.op("dve", lambda: nc.vector.scalar_tensor_tensor(
                            out=cv[:, j, s0 + lo:s0 + hi], in0=glu[:, j, s0 + lo + o:s0 + hi + o], scalar=cw[:, j, k:k + 1],
                            in1=cv[:, j, s0 + lo:s0 + hi], op0=ALU.mult, op1=ALU.add),
                            reads=glu.regs + [cw, cv.regs[j]], writes=[cv.regs[j]])

                    for j in range(4):
                        for (s0, n) in ((0, TC), (TC, T)):
                            conv_ops.append(mk_first(j, s0, n))
                            for k in range(31):
                                o = k - 15
                                if o == 0:
                                    continue
                                conv_ops.append(mk_tap(j, s0, max(0, -o), min(n, n - o), o, k))
                    conv_ops.reverse()
                    self._attention(qT, kT, vaug, mix, conv_ops)
                    while conv_ops:
                        conv_ops.pop()()
                    xb = [kb.sb(f"lnxb{i}", [128, 512], BF16) for i in range(4)]
                    x2 = [kb.sb(f"lnx2{i}", [128, 512], BF16) for i in range(4)]
                    mv = kb.sb("lnmv", [128, 3, 512], F32)
                    tmpc = [kb.sb(f"lntmp{i}", [128, 512], F32) for i in range(1)] * 2
                    for bi, (s0, n) in enumerate(BLOCKS):
                        pm, p2 = PS[0 + 2 * (bi % 2)], PS[1 + 2 * (bi % 2)]
                        for j in range(4):
                            kb.op("act", lambda: nc.scalar.activation(out=xb[j][:, 0:n], in_=cv[:, j, s0:s0 + n], func=AF.Copy),
                                  reads=[cv.regs[j]], writes=[xb[j]])
                            kb.op("act", lambda: nc.scalar.activation(out=x2[j][:, 0:n], in_=cv[:, j, s0:s0 + n], func=AF.Square),
                                  reads=[cv.regs[j]], writes=[x2[j]])
                        for j in range(4):
                            kb.op("pe", lambda: nc.tensor.matmul(pm[:, 0:n], lhsT=self.ones_bf[:], rhs=xb[j][:, 0:n],
                                                                 start=(j == 0), stop=(j == 3)),
                                  reads=[xb[j], self.ones_bf], writes=[pm], signal=(j == 3))
                        for j in range(4):
                            kb.op("pe", lambda: nc.tensor.matmul(p2[:, 0:n], lhsT=self.ones_bf[:], rhs=x2[j][:, 0:n],
                                                                 start=(j == 0), stop=(j == 3)),
                                  reads=[x2[j], self.ones_bf], writes=[p2], signal=(j == 3))
                        kb.op("act", lambda: nc.scalar.activation(out=mv[:, 0, 0:n], in_=pm[:, 0:n], func=AF.Copy, scale=1.0 / 512),
                              reads=[pm], writes=[mv])
                        kb.op("dve", lambda: nc.vector.tensor_tensor(out=mv[:, 1, 0:n], in0=mv[:, 0, 0:n], in1=mv[:, 0, 0:n],
                                                                     op=ALU.mult), reads=[mv], writes=[mv])
                        kb.op("dve", lambda: nc.vector.scalar_tensor_tensor(out=mv[:, 1, 0:n], in0=p2[:, 0:n], scalar=1.0 / 512,
                                                                            in1=mv[:, 1, 0:n], op0=ALU.mult, op1=ALU.subtract),
                              reads=[p2, mv], writes=[mv])
                        kb.op("act", lambda: nc.scalar.activation(out=mv[:, 2, 0:n], in_=mv[:, 1, 0:n], func=AF.Sqrt,
                                                                  bias=self.eps_t[:, 0:1]), reads=[mv, self.eps_t], writes=[mv])
                        kb.op("dve", lambda: nc.vector.reciprocal(out=mv[:, 2, 0:n], in_=mv[:, 2, 0:n]), reads=[mv], writes=[mv])
                        for j in range(4):
                            tc_ = tmpc[j % 2]
                            kb.op("dve", lambda: nc.vector.tensor_tensor(out=tc_[:, 0:n], in0=cv[:, j, s0:s0 + n], in1=mv[:, 0, 0:n],
                                                                         op=ALU.subtract), reads=[cv.regs[j], mv], writes=[tc_])
                            kb.op("dve", lambda: nc.vector.tensor_tensor(out=tc_[:, 0:n], in0=tc_[:, 0:n], in1=mv[:, 2, 0:n],
                                                                         op=ALU.mult), reads=[tc_, mv], writes=[tc_])
                            kb.op("act", lambda: nc.scalar.activation(out=mix[:, 4 + j, s0:s0 + n], in_=tc_[:, 0:n], func=AF.Silu,
                                                                      scale=cb[:, j, 1:2], bias=cb[:, j, 2:3]),
                                  reads=[tc_, cb], writes=[mix.regs[bi]])
                with kb.scope():
                    w_o = kb.sb("w_o", [128, 8, 1024], BF16)
                    wv = inp["ev_w_o"].t[0].rearrange("(kc p) n -> p kc n", p=128)
                    for i in range(2):
                        kb.dma("pool", w_o[:, :, i * 512:(i + 1) * 512], wv[:, :, i * 512:(i + 1) * 512], writes=[w_o])
                    self.proj_residual(src, mix, w_o, 8, gbc, 0, None, res1, A2, modT[:, :, 24:32], hn2T)
            with kb.scope():
                yacc = kb.sb("yacc", [128, NT, D], F32, nreg=NT)
                w1v = inp["ev_ff_w1"].t[0].rearrange("(kc p) n -> p kc n", p=128)
                w3v = inp["ev_ff_w3"].t[0].rearrange("(kc p) n -> p kc n", p=128)
                w2v = inp["ev_ff_w2"].t[0].rearrange("(c p) n -> p c n", p=128)
                fblocks = [(i * 512, 512) for i in range(5)] + [(2560, 256)]
                self.ffn(hn2T, BLOCKS, [(w1v, w3v, w2v, f0, fw, None) for (f0, fw) in fblocks], yacc)
                self.final_residual(res1, yacc, gbc, 1, res_out, list(range(NT)))


    def _attention(self, qT, kT, vaug, mix, conv_ops):
        kb, nc = self.kb, self.nc
        PS = self.PS
        pT = [kb.sb(f"pT{i}", [128, 512], BF16) for i in range(2)]
        rden = [kb.sb(f"rden{i}", [64, 512], F32) for i in range(1)] * 2
        on = [kb.sb(f"on{i}", [64, 512], F32) for i in range(1)] * 2
        cnt = 0
        hc = 0
        for bi, (s0, n) in enumerate(BLOCKS):
            ktiles = range(0, 2) if bi == 0 else range(0, NT)
            for h in range(8):
                g = h // 4
                ch, half = h // 2, h % 2
                po = PS[6 + hc % 2]
                i2 = hc % 2
                hc += 1
                nk = len(ktiles)
                kts = list(ktiles)
                base = cnt
                cnt += nk

                def qk(i):
                    pss_ = PS[(base + i) % 4]
                    kt_ = kts[i]
                    kb.op("pe", lambda: nc.tensor.matmul(
                        pss_[:, 0:n], lhsT=kT[half * 64:(half + 1) * 64, g, kt_ * 128:(kt_ + 1) * 128],
                        rhs=qT[half * 64:(half + 1) * 64, ch, s0:s0 + n], start=True, stop=True),
                        reads=kT.regs + [qT.regs[bi]], writes=[pss_])

                qk(0)
                if nk > 1:
                    qk(1)
                for ki in range(nk):
                    pss = PS[(base + ki) % 4]
                    pt_ = pT[(base + ki) % 2]
                    kt = kts[ki]
                    if conv_ops and (base + ki) % 2 == 0:
                        conv_ops.pop()()
                    kb.op("act", lambda: nc.scalar.activation(out=pt_[:, 0:n], in_=pss[:, 0:n], func=AF.Exp, scale=0.125),
                          reads=[pss], writes=[pt_])
                    if ki + 2 < nk:
                        qk(ki + 2)
                    kb.op("pe", lambda: nc.tensor.matmul(po[:, 0:n], lhsT=vaug[:, kt, g, :], rhs=pt_[:, 0:n],
                                                         start=(ki == 0), stop=(ki == nk - 1)),
                          reads=[pt_, vaug.regs[kt]], writes=[po])
                kb.op("act", lambda: nc.scalar.activation(out=rden[i2][:, 0:n], in_=po[64:128, 0:n], func=AF.Copy),
                      reads=[po], writes=[rden[i2]])
                kb.op("dve", lambda: nc.vector.reciprocal(out=rden[i2][:, 0:n], in_=rden[i2][:, 0:n]),
                      reads=[rden[i2]], writes=[rden[i2]])
                if half == 0:
                    kb.op("dve", lambda: nc.vector.tensor_tensor(out=mix[0:64, ch, s0:s0 + n], in0=po[0:64, 0:n],
                                                                 in1=rden[i2][:, 0:n], op=ALU.mult),
                          reads=[po, rden[i2]], writes=[mix.regs[bi]])
                else:
                    kb.op("dve", lambda: nc.vector.tensor_tensor(out=on[i2][:, 0:n], in0=po[0:64, 0:n],
                                                                 in1=rden[i2][:, 0:n], op=ALU.mult),
                          reads=[po, rden[i2]], writes=[on[i2]])
                    kb.op("act", lambda: nc.scalar.activation(out=mix[64:128, ch, s0:s0 + n], in_=on[i2][:, 0:n],
                                                              func=AF.Copy),
                          reads=[on[i2]], writes=[mix.regs[bi]])

    def proj_residual(self, src, actT, w_sb, nkc, gbc, gi, rowscale, res1, A2, S2, hn2T, tiles=None):
        kb, nc = self.kb, self.nc
        PS = self.PS
        xr = [kb.sb(f"pr_x{i}", [128, D], F32) for i in range(2)]
        tm = [kb.sb(f"pr_t{i}", [128, D], F32) for i in range(2)]
        junk = kb.sb("pr_junk", [128, D], F32)
        xn = [kb.sb(f"pr_xn{i}", [128, D], F32) for i in range(2)]
        ss = [kb.sb(f"pr_ss{i}", [128, 4], F32) for i in range(2)]
        tiles = list(range(NT)) if tiles is None else tiles
        for i, t in enumerate(tiles):
            w = 1 if t < 2 else 0
            i2 = i % 2
            x_ = xr[i2]
            kb.dma("sp", x_[:], self.xtile_src(src, t), writes=[x_])
            for h in range(2):
                po = PS[2 + 2 * i2 + h]
                for kc in range(nkc):
                    kb.op("pe", lambda: nc.tensor.matmul(po[:, :], lhsT=actT[:, kc, t * 128:(t + 1) * 128],
                                                         rhs=w_sb[:, kc, h * 512:(h + 1) * 512], start=(kc == 0),
                                                         stop=(kc == nkc - 1)),
                          reads=[actT] + actT.regs + [w_sb], writes=[po], signal=(kc == nkc - 1))
                if rowscale is not None:
                    kb.op("act", lambda: nc.scalar.activation(out=tm[i2][:, h * 512:(h + 1) * 512], in_=po[:, :], func=AF.Identity,
                                                              scale=rowscale[:, t:t + 1]),
                          reads=[po, rowscale], writes=[tm[i2]])
                    kb.op("dve", lambda: nc.vector.tensor_tensor(out=tm[i2][:, h * 512:(h + 1) * 512], in0=tm[i2][:, h * 512:(h + 1) * 512],
                                                                 in1=gbc[:, w, gi, h * 512:(h + 1) * 512], op=ALU.mult),
                          reads=[tm[i2], gbc], writes=[tm[i2]])
                else:
                    kb.op("dve", lambda: nc.vector.tensor_tensor(out=tm[i2][:, h * 512:(h + 1) * 512], in0=po[:, :],
                                                                 in1=gbc[:, w, gi, h * 512:(h + 1) * 512], op=ALU.mult),
                          reads=[po, gbc], writes=[tm[i2]])
            kb.op("pool", lambda: nc.gpsimd.tensor_tensor(out=x_[:], in0=x_[:], in1=tm[i2][:], op=ALU.add),
                  reads=[x_, tm[i2]], writes=[x_])
            kb.dma("sp", res1[t * 128:(t + 1) * 128, :], x_[:], reads=[x_], writes=[res1.regs[t]])
            self.norm_tile(x_, A2[:, w, :], S2[:, w, :], hn2T, t * 128, (PS[0], PS[1]), (junk, ss[i2], xn[i2]))

    def ffn(self, hnT, blocks, wblocks, yacc, gate_fn=None):
        kb, nc = self.kb, self.nc
        PS = self.PS
        w1b = [kb.sb(f"ffw1_{i}", [128, 8, 512], BF16) for i in range(2)]
        w3b = [kb.sb(f"ffw3_{i}", [128, 8, 512], BF16) for i in range(2)]
        w2b = [kb.sb(f"ffw2_{i}", [128, 4, D], BF16) for i in range(2)]
        sg = [kb.sb(f"ffsg{i}", [128, 512], F32) for i in range(2)]
        gg = [kb.sb(f"ffg{i}", [128, 4, 512], BF16) for i in range(2)]
        it = 0
        ib = 0
        iy = 0
        started = set()
        def issue_loads(wi_):
            w1v_, w3v_, w2v_, f0_, fw_, _g = wblocks[wi_]
            nfc_ = fw_ // 128
            kb.dma("pool", w1b[wi_ % 2][:, :, 0:fw_], w1v_[:, :, f0_:f0_ + fw_], writes=[w1b[wi_ % 2]])
            kb.dma("pool", w3b[wi_ % 2][:, :, 0:fw_], w3v_[:, :, f0_:f0_ + fw_], writes=[w3b[wi_ % 2]])
            kb.dma("pool", w2b[wi_ % 2][:, 0:nfc_, :], w2v_[:, f0_ // 128:f0_ // 128 + nfc_, :], writes=[w2b[wi_ % 2]])

        st = {"iy": 0, "pending": None}
        issue_loads(0)
        for wi, (w1v, w3v, w2v, f0, fw, gate) in enumerate(wblocks):
            b1, b3, b2 = w1b[wi % 2], w3b[wi % 2], w2b[wi % 2]
            nfc = fw // 128
            if st["pending"] is not None:
                st["pending"]()
                st["pending"] = None
            if wi + 1 < len(wblocks):
                issue_loads(wi + 1)
            gbcast = gate_fn(gate) if gate is not None else None
            for (s0, n) in blocks:
                g_ = gg[ib % 2]
                ib += 1
                for c in range(nfc):
                    p1 = PS[it % 2]
                    p3 = PS[2 + it % 2]
                    s_ = sg[it % 2]
                    it += 1
                    for kc in range(8):
                        kb.op("pe", lambda: nc.tensor.matmul(p1[:, 0:n], lhsT=b1[:, kc, c * 128:(c + 1) * 128], rhs=hnT[:, kc, s0:s0 + n],
                                                             start=(kc == 0), stop=(kc == 7)),
                              reads=[b1, hnT] + hnT.regs, writes=[p1], signal=(kc == 7))
                    for kc in range(8):
                        kb.op("pe", lambda: nc.tensor.matmul(p3[:, 0:n], lhsT=b3[:, kc, c * 128:(c + 1) * 128], rhs=hnT[:, kc, s0:s0 + n],
                                                             start=(kc == 0), stop=(kc == 7)),
                              reads=[b3, hnT] + hnT.regs, writes=[p3], signal=(kc == 7))
                    kb.op("act", lambda: nc.scalar.activation(out=s_[:, 0:n], in_=p1[:, 0:n], func=AF.Silu), reads=[p1], writes=[s_])
                    if gbcast is None:
                        kb.op("dve", lambda: nc.vector.tensor_tensor(out=g_[:, c, 0:n], in0=p3[:, 0:n], in1=s_[:, 0:n], op=ALU.mult),
                              reads=[p3, s_], writes=[g_])
                    else:
                        kb.op("dve", lambda: nc.vector.tensor_tensor(out=s_[:, 0:n], in0=p3[:, 0:n], in1=s_[:, 0:n], op=ALU.mult),
                              reads=[p3, s_], writes=[s_])
                        kb.op("pool", lambda: nc.gpsimd.tensor_tensor(out=g_[:, c, 0:n], in0=s_[:, 0:n], in1=gbcast[:, s0:s0 + n], op=ALU.mult),
                              reads=[s_, gbcast], writes=[g_])
                def py_stage(g_=g_, b2=b2, s0=s0, n=n, nfc=nfc):
                    for tl in range(n // 128):
                        t = s0 // 128 + tl
                        for h in range(2):
                            py = PS[4 + st["iy"] % 4]
                            st["iy"] += 1
                            for c in range(nfc):
                                kb.op("pe", lambda: nc.tensor.matmul(py[:, :], lhsT=g_[:, c, tl * 128:(tl + 1) * 128], rhs=b2[:, c, h * 512:(h + 1) * 512],
                                                                     start=(c == 0), stop=(c == nfc - 1)),
                                      reads=[g_, b2], writes=[py], signal=(c == nfc - 1))
                            if (t, h) not in started:
                                started.add((t, h))
                                kb.op("act", lambda: nc.scalar.activation(out=yacc[:, t, h * 512:(h + 1) * 512], in_=py[:, :], func=AF.Copy),
                                      reads=[py], writes=[yacc.regs[t]])
                            else:
                                kb.op("dve", lambda: nc.vector.tensor_tensor(out=yacc[:, t, h * 512:(h + 1) * 512], in0=py[:, :],
                                                                             in1=yacc[:, t, h * 512:(h + 1) * 512], op=ALU.add),
                                      reads=[py, yacc.regs[t]], writes=[yacc.regs[t]])

                if st["pending"] is not None:
                    st["pending"]()
                st["pending"] = py_stage
        if st["pending"] is not None:
            st["pending"]()
            st["pending"] = None

    def final_residual(self, res1, yacc, gbc, gi, res_out, tiles, final_norm=None, lat_only=False):
        kb, nc = self.kb, self.nc
        xr = [kb.sb(f"fr_x{i}", [128, D], F32) for i in range(2)]
        if final_norm is not None:
            fnb = kb.sb("fr_fn", [128, D], F32)
            kb.dma("sp", fnb[:], final_norm[0:1, :].to_broadcast([128, D]), writes=[fnb])
            junk = kb.sb("fr_junk", [128, D], F32)
            ss = [kb.sb(f"fr_ss{i}", [128, 4], F32) for i in range(2)]
        for i, t in enumerate(tiles):
            w = 1 if (t < 2 and not lat_only) else 0
            x_ = xr[i % 2]
            kb.dma("sp", x_[:], res1[t * 128:(t + 1) * 128, :], reads=[res1.regs[t]], writes=[x_])
            kb.op("dve", lambda: nc.vector.tensor_tensor(out=yacc[:, t, :], in0=yacc[:, t, :], in1=gbc[:, w, gi, :], op=ALU.mult),
                  reads=[yacc.regs[t], gbc], writes=[yacc.regs[t]])
            kb.op("pool", lambda: nc.gpsimd.tensor_tensor(out=x_[:], in0=x_[:], in1=yacc[:, t, :], op=ALU.add),
                  reads=[x_, yacc.regs[t]], writes=[x_])
            if final_norm is None:
                kb.dma("sp", res_out[t * 128:(t + 1) * 128, :], x_[:], reads=[x_], writes=[res_out.regs[t]])
            else:
                s_ = ss[i % 2]
                kb.op("act", lambda: nc.scalar.activation(out=junk[:], in_=x_[:], func=AF.Square, accum_out=s_[:, 0:1]),
                      reads=[x_], writes=[junk, s_])
                kb.op("act", lambda: nc.scalar.activation(out=s_[:, 1:2], in_=s_[:, 0:1], func=AF.Sqrt, scale=1.0 / D,
                                                          bias=self.eps_t[:, 0:1]), reads=[s_, self.eps_t], writes=[s_])
                kb.op("dve", lambda: nc.vector.reciprocal(out=s_[:, 2:3], in_=s_[:, 1:2]), reads=[s_], writes=[s_])
                kb.op("dve", lambda: nc.vector.scalar_tensor_tensor(out=x_[:], in0=x_[:], scalar=s_[:, 2:3], in1=fnb[:],
                                                                    op0=ALU.mult, op1=ALU.mult),
                      reads=[x_, s_, fnb], writes=[x_])
                lt = t if lat_only else t - 2
                kb.dma("sp", res_out[lt * 128:(lt + 1) * 128, :], x_[:], reads=[x_], writes=[res_out.regs[lt]])

    def odd_layer(self, src, final_out):
        kb, nc = self.kb, self.nc
        inp = self.inp
        PS = self.PS
        NH = 32
        with kb.scope():
            ada = self.adaln("od", inp["od_norm1"], inp["od_norm2"])
            A1, A2, modT, gbc = ada["A1"], ada["A2"], ada["modT"], ada["gbc"]
            hn2T = kb.sb("ohn2T", [128, 8, T], BF16, nreg=16)
            gates = kb.sb("gates", [128, 16, 8], F32, nreg=16)
            xmid = self.dbg_out("od_xmid", [T, D])
            xmid.regs = [Reg() for _ in range(16)]
            zsT = self.dbg_out("zsT", [16, 128, T], BF16)
            xsT = self.dbg_out("xsT", [16, 128, TT], BF16)
            BT = self.dbg_out("BT", [4, 128, TT], BF16)
            CT = self.dbg_out("CT", [4, 128, TT], BF16)
            xs_tok = self.dbg_out("xs_tok", [NT, 128, 2048], BF16)
            B_tok = self.dbg_out("B_tok", [NT, 128, 512], BF16)
            Sst = self.dbg_out("Sst", [2, NT, 128, 2048], BF16)
            for b_ in (Sst,):
                b_.regs = [Reg() for _ in range(2 * NT)]
            with kb.scope():
                dt_sb = kb.sb("dt_sb", [128, NT, 64], F32)
                a_sb = kb.sb("a_sb", [128, NT, 64], F32)
                with kb.scope():
                    hnT = kb.sb("ohnT", [128, 8, TT], BF16)
                    with kb.scope():
                        xt = [kb.sb(f"oxt{i}", [128, D], F32) for i in range(2)]
                        junk = kb.sb("ojunk", [128, D], BF16)
                        xn = kb.sb("oxn", [128, D], F32)
                        ss = [kb.sb(f"oss{i}", [128, 4], F32) for i in range(2)]
                        for t in range(NT):
                            w = 1 if t < 2 else 0
                            x_ = xt[t % 2]
                            kb.dma("sp", x_[:], self.xtile_src(src, t), reads=[src[2].regs[t]], writes=[x_])
                            self.norm_tile(x_, A1[:, w, :], modT[:, w, 0:8], hnT, t * 128, (PS[0], PS[1]), (junk, ss[t % 2], xn))
                    wv = inp["od_w_in"].t[0].rearrange("(kc p) n -> p kc n", p=128)
                    wb = [kb.sb(f"owb{i}", [128, 8, 512], BF16) for i in range(2)]
                    cw = kb.sb("ocw", [128, 24, 7], F32)
                    cwst = kb.sb("ocwst", [7, 3072], F32)
                    kb.dma("sp", cwst[:], inp["od_conv_w"][0], writes=[cwst])
                    for j_ in range(24):
                        kb.op("pe", lambda: nc.tensor.transpose(PS[7][:, j_ * 7:(j_ + 1) * 7], cwst[0:7, j_ * 128:(j_ + 1) * 128], self.ident[0:7, 0:7]),
                              reads=[cwst, self.ident], writes=[PS[7]], signal=(j_ == 23))
                    kb.op("dve", lambda: nc.vector.tensor_copy(out=cw[:].rearrange("p c k -> p (c k)"), in_=PS[7][:, 0:168]), reads=[PS[7]], writes=[cw])
                    cbias = kb.sb("ocb", [128, 24], F32)
                    self.colT(cbias[:], inp["od_conv_b"][0, :].rearrange("(c p) -> c p", p=128), 24, ps=PS[6], wbuf=cbias)
                    pre_l = [kb.sb(f"opre{i}", [128, TT + 12], BF16) for i in range(2)]
                    for pb_ in pre_l:
                        kb.op("pool", lambda: nc.gpsimd.memset(pb_[:], 0.0), writes=[pb_])
                    dg_l = [kb.sb(f"odiag{i}", [128, 7, 128], BF16) for i in range(2)]
                    pcol = lambda u: 3 + u if u < TC else 9 + u
                    postf_l = [kb.sb(f"opostf{i}", [128, TT], F32) for i in range(2)]
                    postb_l = [kb.sb(f"opostb{i}", [128, TT], BF16) for i in range(2)]
                    zst = [kb.sb(f"ozst{i}", [128, T], BF16) for i in range(2)]
                    tokst = [kb.sb(f"otok{i}", [128, NT, 128], BF16) for i in range(2)]
                    ipp = 0
                    itk = 0
                    def o1_load(blk_):
                        for hh in range(2):
                            kb.dma("pool", wb[blk_ % 2][:, :, hh * 256:(hh + 1) * 256],
                                   wv[:, :, blk_ * 512 + hh * 256:blk_ * 512 + (hh + 1) * 256], writes=[wb[blk_ % 2]])

                    o1_load(0)
                    for blk in range(10):
                        wb_ = wb[blk % 2]
                        c0 = blk * 512
                        if blk + 1 < 10:
                            o1_load(blk + 1)
                        for c in range(4):
                            fc = blk * 4 + c
                            is_z = fc < 16
                            blocks = LAT_BLOCKS if is_z else BLOCKS
                            zs_ = zst[fc % 2]
                            pre, postf, postb, dg = pre_l[fc % 2], postf_l[fc % 2], postb_l[fc % 2], dg_l[fc % 2]
                            for (s0, n) in blocks:
                                pp = PS[2 + ipp % 3]
                                ipp += 1
                                for kc in range(8):
                                    kb.op("pe", lambda: nc.tensor.matmul(pp[:, 0:n], lhsT=wb_[:, kc, c * 128:(c + 1) * 128],
                                                                         rhs=hnT[:, kc, s0:s0 + n], start=(kc == 0), stop=(kc == 7)),
                                          reads=[wb_, hnT], writes=[pp], signal=(kc == 7))
                                if is_z:
                                    kb.op("act", lambda: nc.scalar.activation(out=zs_[:, s0 - TC:s0 - TC + n], in_=pp[:, 0:n], func=AF.Silu),
                                          reads=[pp], writes=[zs_])
                                else:
                                    kb.op("act", lambda: nc.scalar.activation(out=pre[:, pcol(s0):pcol(s0) + n], in_=pp[:, 0:n], func=AF.Copy),
                                          reads=[pp], writes=[pre])
                            if is_z:
                                kb.dma("sp", zsT[fc], zs_[:], reads=[zs_], writes=[zsT])
                                continue
                            cc = fc - 16
                            for k in range(7):
                                kb.op("dve", lambda: nc.vector.tensor_scalar(out=dg[:, k, :], in0=self.ident[:], scalar1=cw[:, cc, k:k + 1],
                                                                             scalar2=None, op0=ALU.mult), reads=[self.ident, cw], writes=[dg])
                            for bi_, (s0, n) in enumerate(BLOCKS):
                                pcv = PS[bi_ % 2]
                                for k in range(7):
                                    kb.op("pe", lambda: nc.tensor.matmul(pcv[:, 0:n], lhsT=dg[:, k, :],
                                                                         rhs=pre[:, pcol(s0) + k - 3:pcol(s0) + k - 3 + n],
                                                                         start=(k == 0), stop=(k == 6)),
                                          reads=[dg, pre], writes=[pcv], signal=(k == 6))
                                kb.op("act", lambda: nc.scalar.activation(out=postf[:, s0:s0 + n], in_=pcv[:, 0:n], func=AF.Silu,
                                                                          bias=cbias[:, cc:cc + 1]), reads=[pcv, cbias], writes=[postf])
                            kb.op("pool", lambda: nc.gpsimd.tensor_copy(out=postb[:], in_=postf[:]), reads=[postf], writes=[postb])
                            if cc < 16:
                                kb.dma("sp", xsT[cc], postb[:], reads=[postb], writes=[xsT])
                            elif cc < 20:
                                kb.dma("sp", BT[cc - 16], postb[:], reads=[postb], writes=[BT])
                            else:
                                kb.dma("sp", CT[cc - 20], postb[:], reads=[postb], writes=[CT])
                            if cc < 20:
                                tk = tokst[itk % 2]
                                itk += 1
                                for t4 in range(0, NT, 4):
                                    nt4 = min(4, NT - t4)
                                    ptr = PS[5 + (t4 // 4) % 3]
                                    for q_ in range(nt4):
                                        t = t4 + q_
                                        kb.op("pe", lambda: nc.tensor.transpose(ptr[:, q_ * 128:(q_ + 1) * 128], postf[:, t * 128:(t + 1) * 128],
                                                                                self.ident[:]),
                                              reads=[postf, self.ident], writes=[ptr], signal=(q_ == nt4 - 1))
                                    kb.op("act", lambda: nc.scalar.activation(
                                        out=tk[:, t4:t4 + nt4, :], in_=ptr[:, 0:nt4 * 128].rearrange("p (t f) -> p t f", f=128), func=AF.Copy),
                                        reads=[ptr], writes=[tk])
                                if cc < 16:
                                    kb.dma("sp", xs_tok[:, :, cc * 128:(cc + 1) * 128].rearrange("t p f -> p t f"), tk[:], reads=[tk], writes=[xs_tok])
                                else:
                                    g_ = cc - 16
                                    kb.dma("sp", B_tok[:, :, g_ * 128:(g_ + 1) * 128].rearrange("t p f -> p t f"), tk[:], reads=[tk], writes=[B_tok])
                    wdt = kb.sb("owdt", [128, 8, 64], BF16)
                    kb.dma("pool", wdt[:], wv[:, :, 5120:5184], writes=[wdt])
                    dtb = kb.sb("odtb", [128, 64], F32)
                    Abc = kb.sb("oAbc", [128, 64], F32)
                    for di, (nb, na) in enumerate((("od_dt_bias_f", "od_a_log_f"), ("od_dt_bias_b", "od_a_log_b"))):
                        kb.dma("sp", dtb[:, di * 32:(di + 1) * 32], inp[nb][0:1, :].to_broadcast([128, 32]), writes=[dtb])
                        kb.dma("sp", Abc[:, di * 32:(di + 1) * 32], inp[na][0:1, :].to_broadcast([128, 32]), writes=[Abc])
                    kb.op("act", lambda: nc.scalar.activation(out=Abc[:], in_=Abc[:], func=AF.Exp), reads=[Abc], writes=[Abc])
                    kb.op("dve", lambda: nc.vector.tensor_scalar(out=Abc[:], in0=Abc[:], scalar1=-1.0, scalar2=None, op0=ALU.mult),
                          reads=[Abc], writes=[Abc])
                    for t in range(NT):
                        pd = PS[t % 2]
                        for kc in range(8):
                            kb.op("pe", lambda: nc.tensor.matmul(pd[:, 0:64], lhsT=hnT[:, kc, t * 128:(t + 1) * 128], rhs=wdt[:, kc, :],
                                                                 start=(kc == 0), stop=(kc == 7)),
                                  reads=[hnT, wdt], writes=[pd], signal=(kc == 7))
                        kb.op("dve", lambda: nc.vector.tensor_tensor(out=dt_sb[:, t, :], in0=pd[:, 0:64], in1=dtb[:], op=ALU.add),
                              reads=[pd, dtb], writes=[dt_sb])
                    kb.op("act", lambda: nc.scalar.activation(out=dt_sb[:], in_=dt_sb[:], func=AF.Exp), reads=[dt_sb], writes=[dt_sb])
                    kb.op("dve", lambda: nc.vector.tensor_scalar(out=dt_sb[:], in0=dt_sb[:], scalar1=1.0, scalar2=None, op0=ALU.add),
                          reads=[dt_sb], writes=[dt_sb])
                    kb.op("act", lambda: nc.scalar.activation(out=dt_sb[:], in_=dt_sb[:], func=AF.Ln), reads=[dt_sb], writes=[dt_sb])
                    kb.op("dve", lambda: nc.vector.tensor_tensor(out=a_sb[:], in0=dt_sb[:], in1=Abc[:].unsqueeze(1).to_broadcast([128, NT, 64]),
                                                                 op=ALU.mult), reads=[dt_sb, Abc], writes=[a_sb])
                if "od_dt" in self.debug:
                    dd = self.dbg_out("od_dt", [128, NT, 64])
                    kb.dma("sp", dd[:], dt_sb[:], reads=[dt_sb], writes=[dd])
                if "stop_o1" not in self.debug:
                    self.ssd(src, dt_sb, a_sb, zsT, xsT, BT, CT, xs_tok, B_tok, Sst, gbc, A2, modT, hn2T, gates, xmid)
            if not (self.debug & {"stop_o1", "stop_ssdA", "stop_ssd"}):
                self.moe(hn2T, gates, xmid, gbc, final_out)

    def ssd(self, src, dt_sb, a_sb, zsT, xsT, BT, CT, xs_tok, B_tok, Sst, gbc, A2, modT, hn2T, gates, xmid):
        kb, nc = self.kb, self.nc
        inp = self.inp
        PS = self.PS
        with kb.scope():
            triu = kb.sb("triu", [128, 128], F32)
            tril = kb.sb("tril", [128, 128], F32)
            ones_f = kb.sb("ones_f", [128, 128], F32)
            kb.dma("sp", triu[:], inp["cst_triu"][:], writes=[triu])
            kb.dma("sp", tril[:], inp["cst_tril"][:], writes=[tril])
            kb.dma("sp", ones_f[:], inp["cst_ones"][:], writes=[ones_f])
            TRI = (triu, tril)
            with kb.scope():
                S = [kb.sb(f"S{i}", [128, 2048], F32) for i in range(2)]
                Sbf = [kb.sb(f"Sbf{i}", [128, 2048], BF16) for i in range(2)]
                xtk = [kb.sb(f"axtk{i}", [128, 2048], BF16) for i in range(2)]
                btk = [kb.sb(f"abtk{i}", [128, 512], BF16) for i in range(2)]
                xw = [kb.sb(f"axw{i}", [128, 2048], BF16) for i in range(2)]
                sm = [kb.sb(f"asm{i}", [128, 4, 32], F32) for i in range(2)]
                it = 0
                orders = [list(range(NT)), [1, 0] + list(range(NT - 1, 1, -1))]
                for d in range(2):
                    kb.op("dve", lambda: nc.vector.memset(S[d][:], 0.0), writes=[S[d]])
                for step in range(NT):
                    for d in range(2):
                        t = orders[d][step]
                        i2 = it % 2
                        it += 1
                        if t >= 2:
                            kb.op("act", lambda: nc.scalar.activation(out=Sbf[i2][:], in_=S[d][:], func=AF.Copy), reads=[S[d]], writes=[Sbf[i2]])
                            kb.dma("sp", Sst[d, t], Sbf[i2][:], reads=[Sbf[i2]], writes=[Sst.regs[d * NT + t]])
                        kb.dma("sp", xtk[i2][:], xs_tok[t], reads=[xs_tok], writes=[xtk[i2]])
                        kb.dma("sp", btk[i2][:], B_tok[t], reads=[B_tok], writes=[btk[i2]])
                        a_t = a_sb[:, t, d * 32:(d + 1) * 32]
                        pc = PS[0]
                        kb.op("pe", lambda: nc.tensor.matmul(pc[:, 0:32], lhsT=TRI[d][:], rhs=a_t, start=True, stop=True),
                              reads=[TRI[d], a_sb], writes=[pc], signal=False)
                        kb.op("pe", lambda: nc.tensor.matmul(pc[:, 32:64], lhsT=ones_f[:], rhs=a_t, start=True, stop=True),
                              reads=[ones_f, a_sb], writes=[pc])
                        m_ = sm[i2]
                        kb.op("act", lambda: nc.scalar.activation(out=m_[:, 0, :], in_=pc[:, 32:64], func=AF.Copy), reads=[pc], writes=[m_])
                        kb.op("dve", lambda: nc.vector.tensor_tensor(out=m_[:, 1, :], in0=m_[:, 0, :], in1=pc[:, 0:32], op=ALU.subtract),
                              reads=[m_, pc], writes=[m_])
                        kb.op("act", lambda: nc.scalar.activation(out=m_[:, 2, :], in_=m_[:, 1, :], func=AF.Exp), reads=[m_], writes=[m_])
                        kb.op("dve", lambda: nc.vector.tensor_tensor(out=m_[:, 2, :], in0=m_[:, 2, :], in1=dt_sb[:, t, d * 32:(d + 1) * 32],
                                                                     op=ALU.mult), reads=[m_, dt_sb], writes=[m_])
                        kb.op("act", lambda: nc.scalar.activation(out=m_[:, 3, :], in_=m_[:, 0, :], func=AF.Exp), reads=[m_], writes=[m_])
                        kb.op("dve", lambda: nc.vector.tensor_tensor(
                            out=xw[i2][:].rearrange("p (h e) -> p h e", e=64), in0=xtk[i2][:].rearrange("p (h e) -> p h e", e=64),
                            in1=m_[:, 2, :].unsqueeze(2).to_broadcast([128, 32, 64]), op=ALU.mult),
                            reads=[xtk[i2], m_], writes=[xw[i2]])
                        for g in range(4):
                            kb.op("pe", lambda: nc.tensor.matmul(PS[1 + g][:, :], lhsT=btk[i2][:, g * 128:(g + 1) * 128],
                                                                 rhs=xw[i2][:, g * 512:(g + 1) * 512], start=True, stop=True),
                                  reads=[btk[i2], xw[i2]], writes=[PS[1 + g]])
                        kb.op("pool", lambda: nc.gpsimd.tensor_tensor(
                            out=S[d][:].rearrange("p (h e) -> p h e", e=64), in0=S[d][:].rearrange("p (h e) -> p h e", e=64),
                            in1=m_[:, 3, :].unsqueeze(2).to_broadcast([128, 32, 64]), op=ALU.mult),
                            reads=[S[d], m_], writes=[S[d]])
                        for g in range(4):
                            kb.op("dve", lambda: nc.vector.tensor_tensor(out=S[d][:, g * 512:(g + 1) * 512], in0=PS[1 + g][:, :],
                                                                         in1=S[d][:, g * 512:(g + 1) * 512], op=ALU.add),
                                  reads=[PS[1 + g], S[d]], writes=[S[d]])
            if "stop_ssdA" in self.debug:
                return
            with kb.scope():
                w_out = kb.sb("w_out", [128, 16, D], BF16)
                wov = inp["od_w_out"].t[0].rearrange("(kc p) n -> p kc n", p=128)
                for i in range(4):
                    kb.dma("pool", w_out[:, i * 4:(i + 1) * 4, :], wov[:, i * 4:(i + 1) * 4, :], writes=[w_out])
                dsk = kb.sb("dsk", [128, 16], F32)
                dsv = inp["od_d_skip"][0, :].rearrange("(c two) -> two c", two=2)
                for hf_ in range(2):
                    kb.dma("sp", dsk[hf_ * 64:(hf_ + 1) * 64, :], dsv[hf_:hf_ + 1, :].to_broadcast([64, 16]), writes=[dsk],
                           allow_slow_non_contiguous=True)
                gn = kb.sb("gn", [128, 16], F32)
                self.colT(gn[:], inp["od_gnorm"][0, :].rearrange("(c p) -> c p", p=128), 16, ps=PS[6], wbuf=gn)
                wr = kb.sb("wr", [128, 8, 32], F32)
                kb.op("dve", lambda: nc.vector.memset(wr[:], 0.0), writes=[wr])
                kb.dma("sp", wr[:, :, 0:8], inp["od_router"][0].rearrange("(kc p) e -> p kc e", p=128), writes=[wr], allow_slow_non_contiguous=True)
                xtk = kb.sb("bxtk", [128, 2048], BF16)
                Sd = [kb.sb(f"bS{i}", [128, 2048], BF16) for i in range(2)]
                BTt = kb.sb("bBT", [128, 4, 128], BF16)
                CTt = kb.sb("bCT", [128, 4, 128], BF16)
                xsTt = kb.sb("bxsT", [128, 16, 128], BF16)
                zsTt = kb.sb("bzsT", [128, 16, 128], BF16)
                cbm = kb.sb("cbm", [128, 2, 4, 128], BF16)
                acs = kb.sb("acs", [128, 2, 32], F32)
                Rb = kb.sb("Rb", [128, 16, 128], F32)
                Dm = [Rb, kb.sb("Dm1", [128, 16, 128], F32)]
                bc = kb.sb("bc", [128, 2, 32, 128], F32)
                Eb = [kb.sb(f"Eb{i}", [128, 128], F32) for i in range(4)]
                xdt = [kb.sb(f"bxdt{i}", [128, 2048], BF16) for i in range(2)]
                E_ = [kb.sb(f"E{i}", [128, 128], F32) for i in range(4)]
                MT = [kb.sb(f"MT{i}", [128, 128], BF16) for i in range(4)]
                Cs = [kb.sb(f"Cs{i}", [128, 128], BF16) for i in range(4)]
                v1 = [kb.sb(f"v1{i}", [128, 128], F32) for i in range(2)]
                v2 = [kb.sb(f"v2{i}", [128, 128], F32) for i in range(2)]
                sq = [kb.sb(f"bsq{i}", [128, 128], BF16) for i in range(2)]
                vgT = kb.sb("vgT", [128, 16, 128], BF16)
                rs = kb.sb("brs", [128, 2], F32)
                xr = kb.sb("bxr", [128, D], F32)
                tm = kb.sb("btm", [128, D], F32)
                junk = kb.sb("bjunk", [128, D], BF16)
                xn = kb.sb("bxn", [128, D], F32)
                ss = kb.sb("bss", [128, 4], F32)
                hf = kb.sb("bhf", [128, 8, 128], F32)
                lg = kb.sb("blg", [128, 8], F32)
                g8 = kb.sb("bg8", [128, 4, 8], F32)
                gs = kb.sb("bgs", [128, 4], F32)
                ih = 0
                stage = 99
                for d_ in self.debug:
                    if d_.startswith("stopb_"):
                        stage = int(d_[6:])
                for t in range(2, NT if stage == 99 else 3):
                    lt = t - 2
                    kb.dma("sp", xtk[:], xs_tok[t], reads=[xs_tok], writes=[xtk])
                    for d in range(2):
                        kb.op("pool", lambda: nc.gpsimd.tensor_tensor(
                            out=xdt[d][:].rearrange("p (h e) -> p h e", e=64), in0=xtk[:].rearrange("p (h e) -> p h e", e=64),
                            in1=dt_sb[:, t, d * 32:(d + 1) * 32].unsqueeze(2).to_broadcast([128, 32, 64]), op=ALU.mult),
                            reads=[xtk, dt_sb], writes=[xdt[d]])
                    for d in range(2):
                        kb.dma("sp", Sd[d][:], Sst[d, t], reads=[Sst.regs[d * NT + t]], writes=[Sd[d]])
                    kb.dma("sp", BTt[:], BT[:, :, t * 128:(t + 1) * 128].rearrange("g p l -> p g l"), reads=[BT], writes=[BTt])
                    kb.dma("sp", CTt[:], CT[:, :, t * 128:(t + 1) * 128].rearrange("g p l -> p g l"), reads=[CT], writes=[CTt])
                    kb.dma("sp", xsTt[:], xsT[:, :, t * 128:(t + 1) * 128].rearrange("c p l -> p c l"), reads=[xsT], writes=[xsTt])
                    kb.dma("sp", zsTt[:], zsT[:, :, lt * 128:(lt + 1) * 128].rearrange("c p l -> p c l"), reads=[zsT], writes=[zsTt])
                    if stage < 2:
                        continue
                    pcb = PS[4]
                    for g in range(4):
                        kb.op("pe", lambda: nc.tensor.matmul(pcb[:, g * 128:(g + 1) * 128], lhsT=BTt[:, g, :], rhs=CTt[:, g, :], start=True, stop=True),
                              reads=[BTt, CTt], writes=[pcb], signal=(g == 3))
                    for d in range(2):
                        kb.op("dve", lambda: nc.vector.tensor_tensor(out=cbm[:, d, :, :], in0=pcb[:, :].rearrange("p (g l) -> p g l", l=128),
                                                                     in1=TRI[d][:].unsqueeze(1).to_broadcast([128, 4, 128]), op=ALU.mult),
                              reads=[pcb, TRI[d]], writes=[cbm])
                    pc = PS[7]
                    for d in range(2):
                        a_t = a_sb[:, t, d * 32:(d + 1) * 32]
                        kb.op("pe", lambda: nc.tensor.matmul(pc[:, d * 32:(d + 1) * 32], lhsT=TRI[d][:], rhs=a_t, start=True, stop=True),
                              reads=[TRI[d], a_sb], writes=[pc])
                    kb.op("act", lambda: nc.scalar.activation(out=acs[:].rearrange("p d h -> p (d h)"), in_=pc[:, 0:64], func=AF.Copy, scale=-1.0),
                          reads=[pc], writes=[acs])
                    ib = 0
                    for d in range(2):
                        for hh in range(2):
                            kb.op("pool", lambda: nc.gpsimd.tensor_tensor(
                                out=Rb[:], in0=a_sb[:, t, d * 32 + hh * 16:d * 32 + (hh + 1) * 16].unsqueeze(2).to_broadcast([128, 16, 128]),
                                in1=TRI[d][:].unsqueeze(1).to_broadcast([128, 16, 128]), op=ALU.mult),
                                reads=[a_sb, TRI[d], Rb], writes=[Rb])
                            for q_ in range(4):
                                pb = PS[5 + ib % 2]
                                ib += 1
                                kb.op("pe", lambda: nc.tensor.matmul(pb[:, :], lhsT=ones_f[:], rhs=Rb[:, q_ * 4:(q_ + 1) * 4, :].rearrange("p h l -> p (h l)"),
                                                                     start=True, stop=True), reads=[ones_f, Rb], writes=[pb])
                                kb.op("act", lambda: nc.scalar.activation(
                                    out=bc[:, d, hh * 16 + q_ * 4:hh * 16 + (q_ + 1) * 4, :], in_=pb[:, :].rearrange("p (h l) -> p h l", l=128), func=AF.Copy),
                                    reads=[pb], writes=[bc])
                    if stage < 3:
                        continue
                    for hh in range(2):
                        for d in range(2):
                            kb.op("dve", lambda: nc.vector.tensor_tensor(
                                out=Dm[d][:], in0=bc[:, d, hh * 16:(hh + 1) * 16, :],
                                in1=acs[:, d, hh * 16:(hh + 1) * 16].unsqueeze(2).to_broadcast([128, 16, 128]), op=ALU.add),
                                reads=[bc, acs], writes=[Dm[d]])
                            kb.op("dve", lambda: nc.vector.tensor_scalar(out=Dm[d][:], in0=Dm[d][:], scalar1=0.0, scalar2=None, op0=ALU.min),
                                  reads=[Dm[d]], writes=[Dm[d]])
                        for hl in range(16):
                            h = hh * 16 + hl
                            g = h // 8
                            ybank = PS[h // 8]
                            yreg = ybank[(h % 2) * 64:(h % 2) * 64 + 64, ((h // 2) % 4) * 128:((h // 2) % 4) * 128 + 128]
                            for d in range(2):
                                i2 = ih % 4
                                i3 = ih % 4
                                ih += 1
                                kb.op("act", lambda: nc.scalar.activation(out=E_[i2][:], in_=Dm[d][:, hl, :], func=AF.Exp),
                                      reads=[Dm[d]], writes=[E_[i2]])
                                kb.op("dve", lambda: nc.vector.tensor_tensor(out=MT[i3][:], in0=E_[i2][:], in1=cbm[:, d, g, :], op=ALU.mult),
                                      reads=[E_[i2], cbm], writes=[MT[i3]])
                                kb.op("act", lambda: nc.scalar.activation(out=Eb[i2][:], in_=bc[:, d, h, :], func=AF.Exp), reads=[bc], writes=[Eb[i2]])
                                kb.op("pool", lambda: nc.gpsimd.tensor_tensor(out=Cs[i3][:], in0=CTt[:, g, :], in1=Eb[i2][:], op=ALU.mult),
                                      reads=[CTt, Eb[i2]], writes=[Cs[i3]])
                                kb.op("pe", lambda: nc.tensor.matmul(yreg, lhsT=xdt[d][:, h * 64:(h + 1) * 64], rhs=MT[i3][:], start=(d == 0), stop=False),
                                      reads=[xdt[d], MT[i3]], writes=[ybank], signal=False)
                                kb.op("pe", lambda: nc.tensor.matmul(yreg, lhsT=Sd[d][:, h * 64:(h + 1) * 64], rhs=Cs[i3][:], start=False, stop=(d == 1)),
                                      reads=[Sd[d], Cs[i3]], writes=[ybank])
                    if stage < 4:
                        continue
                    pss = PS[7]
                    for fc in range(16):
                        i2 = fc % 2
                        yr = PS[fc // 4][:, (fc % 4) * 128:(fc % 4 + 1) * 128]
                        kb.op("dve", lambda: nc.vector.scalar_tensor_tensor(out=v1[i2][:], in0=xsTt[:, fc, :], scalar=dsk[:, fc:fc + 1], in1=yr,
                                                                            op0=ALU.mult, op1=ALU.add),
                              reads=[xsTt, dsk, PS[fc // 4]], writes=[v1[i2]])
                        kb.op("pool", lambda: nc.gpsimd.tensor_tensor(out=v2[i2][:], in0=v1[i2][:], in1=zsTt[:, fc, :], op=ALU.mult),
                              reads=[v1[i2], zsTt], writes=[v2[i2]])
                        kb.op("act", lambda: nc.scalar.activation(out=sq[i2][:], in_=v2[i2][:], func=AF.Square), reads=[v2[i2]], writes=[sq[i2]])
                        kb.op("act", lambda: nc.scalar.activation(out=vgT[:, fc, :], in_=v2[i2][:], func=AF.Identity, scale=gn[:, fc:fc + 1]),
                              reads=[v2[i2], gn], writes=[vgT])
                        kb.op("pe", lambda: nc.tensor.matmul(pss[:, 64:66], lhsT=sq[i2][:], rhs=self.ones_bf[:, 0:2], start=(fc == 0), stop=(fc == 15)),
                              reads=[sq[i2], self.ones_bf], writes=[pss])
                    kb.op("act", lambda: nc.scalar.activation(out=rs[:, 0:1], in_=pss[:, 64:65], func=AF.Sqrt, scale=1.0 / 2048, bias=self.eps_t[:, 0:1]),
                          reads=[pss, self.eps_t], writes=[rs])
                    kb.op("dve", lambda: nc.vector.reciprocal(out=rs[:, 1:2], in_=rs[:, 0:1]), reads=[rs], writes=[rs])
                    kb.dma("sp", xr[:], self.xtile_src(src, t), reads=[src[2].regs[t]], writes=[xr])
                    for hlf in range(2):
                        po = PS[5 + hlf]
                        for fc in range(16):
                            kb.op("pe", lambda: nc.tensor.matmul(po[:, :], lhsT=vgT[:, fc, :], rhs=w_out[:, fc, hlf * 512:(hlf + 1) * 512],
                                                                 start=(fc == 0), stop=(fc == 15)), reads=[vgT, w_out], writes=[po], signal=(fc == 15))
                        kb.op("act", lambda: nc.scalar.activation(out=tm[:, hlf * 512:(hlf + 1) * 512], in_=po[:, :], func=AF.Identity, scale=rs[:, 1:2]),
                              reads=[po, rs], writes=[tm])
                    kb.op("dve", lambda: nc.vector.tensor_tensor(out=tm[:], in0=tm[:], in1=gbc[:, 0, 0, :], op=ALU.mult), reads=[tm, gbc], writes=[tm])
                    kb.op("pool", lambda: nc.gpsimd.tensor_tensor(out=xr[:], in0=xr[:], in1=tm[:], op=ALU.add), reads=[xr, tm], writes=[xr])
                    kb.dma("sp", xmid[lt * 128:(lt + 1) * 128, :], xr[:], reads=[xr], writes=[xmid.regs[lt]])
                    if stage < 5:
                        continue
                    plg = PS[5]
                    self.norm_tile(xr, A2[:, 0, :], modT[:, 0, 24:32], hn2T, lt * 128, (PS[4], PS[7]), (junk, ss, xn), router=None if 'norouter' in self.debug else (wr, hf, plg))
                    kb.op("act", lambda: nc.scalar.activation(out=lg[:], in_=plg[:, 0:8], func=AF.Copy), reads=[plg], writes=[lg])
                    if "od_logits" in self.debug:
                        if lt == 0:
                            self._dlg = self.dbg_out("od_logits", [T, 8])
                        kb.dma("sp", self._dlg[lt * 128:(lt + 1) * 128, :], lg[:], reads=[lg], writes=[])
                    if stage < 6:
                        continue
                    kb.op("dve", lambda: nc.vector.max(out=g8[:, 0, :], in_=lg[:]), reads=[lg], writes=[g8])
                    kb.op("dve", lambda: nc.vector.tensor_scalar(out=g8[:, 1, :], in0=lg[:], scalar1=g8[:, 0, 1:2], scalar2=None, op0=ALU.is_ge),
                          reads=[lg, g8], writes=[g8])
                    kb.op("dve", lambda: nc.vector.tensor_scalar(out=gs[:, 0:1], in0=g8[:, 0, 0:1], scalar1=-1.0, scalar2=None, op0=ALU.mult),
                          reads=[g8], writes=[gs])
                    kb.op("act", lambda: nc.scalar.activation(out=g8[:, 2, :], in_=lg[:], func=AF.Exp, bias=gs[:, 0:1]), reads=[lg, gs], writes=[g8])
                    kb.op("dve", lambda: nc.vector.tensor_tensor(out=g8[:, 3, :], in0=g8[:, 2, :], in1=g8[:, 1, :], op=ALU.mult), reads=[g8], writes=[g8])
                    kb.op("dve", lambda: nc.vector.tensor_reduce(out=gs[:, 1:2], in_=g8[:, 3, :], axis=AX.X, op=ALU.add), reads=[g8], writes=[gs])
                    kb.op("dve", lambda: nc.vector.reciprocal(out=gs[:, 2:3], in_=gs[:, 1:2]), reads=[gs], writes=[gs])
                    kb.op("dve", lambda: nc.vector.tensor_scalar(out=gates[:, lt, :], in0=g8[:, 3, :], scalar1=gs[:, 2:3], scalar2=None, op0=ALU.mult),
                          reads=[g8, gs], writes=[gates.regs[lt]])

    def moe(self, hn2T, gates, xmid, gbc, final_out):
        kb, nc = self.kb, self.nc
        inp = self.inp
        PS = self.PS
        with kb.scope():
            yacc = kb.sb("myacc", [128, 16, D], F32, nreg=16)
            gT = kb.sb("gT", [8, T], F32)
            sel8 = kb.sb("sel8", [8, 8, 128], F32)
            kb.dma("sp", sel8[:].rearrange("k e m -> k (e m)"), inp["cst_sel8"][:], writes=[sel8])
            for t4 in range(0, 16, 4):
                pt = PS[(t4 // 4) % 2]
                for q_ in range(4):
                    kb.op("pe", lambda: nc.tensor.transpose(pt[0:8, q_ * 128:(q_ + 1) * 128], gates[:, t4 + q_, :], self.ident[:]),
                          reads=[gates.regs[t4 + q_], self.ident], writes=[pt], signal=(q_ == 3))
                kb.op("act", lambda: nc.scalar.activation(out=gT[:, t4 * 128:(t4 + 4) * 128], in_=pt[0:8, :], func=AF.Copy), reads=[pt], writes=[gT])
            with kb.scope():
                gbcast = kb.sb("gbcast", [128, T], F32)
                state = {"e": None}

                def gate_fn(e):
                    if state["e"] != e:
                        state["e"] = e
                        for b4 in range(4):
                            pg = PS[6 + b4 % 2]
                            kb.op("pe", lambda: nc.tensor.matmul(pg[:, :], lhsT=sel8[:, e, :], rhs=gT[:, b4 * 512:(b4 + 1) * 512], start=True, stop=True),
                                  reads=[sel8, gT], writes=[pg])
                            kb.op("act", lambda: nc.scalar.activation(out=gbcast[:, b4 * 512:(b4 + 1) * 512], in_=pg[:, :], func=AF.Copy),
                                  reads=[pg], writes=[gbcast])
                    return gbcast

                wblocks = []
                for e in range(NEXP):
                    w1v = inp["od_ex_w1"].t[0, e].rearrange("(kc p) n -> p kc n", p=128)
                    w3v = inp["od_ex_w3"].t[0, e].rearrange("(kc p) n -> p kc n", p=128)
                    w2v = inp["od_ex_w2"].t[0, e].rearrange("(c p) n -> p c n", p=128)
                    for fb in range(D_FFE // 512):
                        wblocks.append((w1v, w3v, w2v, fb * 512, 512, e))
                self.ffn(hn2T, [(512 * i, 512) for i in range(4)], wblocks, yacc, gate_fn)
            self.final_residual(xmid, yacc, gbc, 1, final_out, list(range(16)), final_norm=self.inp["final_norm"], lat_only=True)


_CACHE = {}


def _build(shapes):
    P = Prog(shapes)
    P.load_consts()
    x1 = P.kb.dram("scr_x1", [TT, D], F32, "Internal", nreg=NT)
    P.even_layer((P.inp["ctx"].t, P.inp["x"].t), x1)
    P.odd_layer((x1.t[0:TC], x1.t[TC:], x1), P.out)
    P.kb.barrier()
    return P


def kernel(**inputs):
    consts = host_consts()
    f32 = lambda a: np.ascontiguousarray(np.asarray(a, dtype=np.float32))
    shared = {}
    for n in WEIGHT_NAMES:
        a = f32(inputs[n])
        shared[n] = a if a.ndim > 1 else a[None, :]
    shared["c_ctx"] = f32(inputs["c_ctx"])[None, :]
    shared.update(consts)
    x = f32(inputs["x"])
    ctx = f32(inputs["ctx"])
    c = f32(inputs["c"])
    nb = x.shape[0]
    in_maps = []
    for b in range(nb):
        m = {"x": x[b], "ctx": ctx[b], "c": c[b:b + 1]}
        m.update(shared)
        in_maps.append(m)
    shapes = {k: list(v.shape) for k, v in in_maps[0].items()}
    P = _build(shapes)
    res = run_bass_kernel_spmd(P.nc, in_maps, core_ids=list(range(nb)))
    return np.stack([np.asarray(r["out"], dtype=np.float32) for r in res.results], axis=0)
```
